# Optimizing a Trainium2 kernel written in Bass

```python
import math
import jax, jax.numpy as jnp
from jax import lax
import numpy as np

D_MODEL = 1024
BATCH = 4
SEQ = 8192
DEPTH = 2

HEAD_DIM = 64
BRANCH_WIDTH = D_MODEL // 2
N_BRANCH = 3
BLOCK = 128
A_HEADS = BRANCH_WIDTH // HEAD_DIM
A_KV_HEADS = 2
WINDOW = 128
B_HEADS = BRANCH_WIDTH // HEAD_DIM
B_KV_HEADS = 2
IDX_HEADS = 4
IDX_DIM = 32
TOPK_MAX = 256
C_HEADS = BRANCH_WIDTH // HEAD_DIM
FORGET_BIAS_INIT = 4.0
N_BUCKETS = 32
MAX_DISTANCE = 512
EPS = 1e-6
ATTN_SCALE = HEAD_DIM ** -0.5

A_SIZES = (A_HEADS * HEAD_DIM, A_KV_HEADS * HEAD_DIM, A_KV_HEADS * HEAD_DIM, BRANCH_WIDTH)
B_SIZES = (B_HEADS * HEAD_DIM, B_KV_HEADS * HEAD_DIM, B_KV_HEADS * HEAD_DIM,
           IDX_HEADS * IDX_DIM, IDX_DIM, IDX_HEADS, BRANCH_WIDTH)
C_SIZES = (C_HEADS * HEAD_DIM, C_HEADS * HEAD_DIM, C_HEADS * HEAD_DIM, C_HEADS, BRANCH_WIDTH)
MERGE_SIZES = (N_BRANCH * D_MODEL,)
IN_SIZES = A_SIZES + B_SIZES + C_SIZES + MERGE_SIZES
IN_COLS = sum(IN_SIZES)

kernel_name = "hybrid_swa_dsa_fox_gated_merge"


def _rms_norm(x, g):
    x32 = x.astype(jnp.float32)
    y = x32 * lax.rsqrt(jnp.mean(x32 * x32, axis=-1, keepdims=True) + EPS)
    return (y * g.astype(jnp.float32)).astype(x.dtype)


def _split(z, sizes):
    outs, start = [], 0
    for s in sizes:
        outs.append(z[..., start:start + s])
        start += s
    return outs


def _t5_bucket(delta):
    n = jnp.maximum(delta, 0)
    max_exact = N_BUCKETS // 2
    nf = jnp.maximum(n, 1).astype(jnp.float32)
    large = max_exact + (jnp.log(nf / max_exact) / math.log(MAX_DISTANCE / max_exact)
                         * (N_BUCKETS - max_exact)).astype(jnp.int32)
    large = jnp.minimum(large, N_BUCKETS - 1)
    return jnp.where(n < max_exact, n, large)


def _to_blocks(a, nb):
    return a.reshape(a.shape[0], nb, BLOCK, *a.shape[2:]).swapaxes(0, 1)


def _sliding_window_attention(q, k, v, sinks, bias_table):
    b_, s_, h_, dh = q.shape
    g_ = h_ // A_KV_HEADS
    nb = s_ // BLOCK
    qb = q.reshape(b_, nb, BLOCK, A_KV_HEADS, g_, dh)
    kb = k.reshape(b_, nb, BLOCK, A_KV_HEADS, dh)
    vb = v.reshape(b_, nb, BLOCK, A_KV_HEADS, dh)
    pad = ((0, 0), (1, 0), (0, 0), (0, 0), (0, 0))
    kk = jnp.concatenate([jnp.pad(kb, pad)[:, :-1], kb], axis=2)
    vv = jnp.concatenate([jnp.pad(vb, pad)[:, :-1], vb], axis=2)
    logits = jnp.einsum('bnqhgd,bnkhd->bnhgqk', qb, kk).astype(jnp.float32) * ATTN_SCALE
    qi = jnp.arange(BLOCK)[:, None]
    ki = jnp.arange(2 * BLOCK)[None, :]
    delta = qi + BLOCK - ki
    band = (delta >= 0) & (delta < WINDOW)
    not_pad = (jnp.arange(nb)[:, None, None] > 0) | (ki >= BLOCK)[None]
    mask = band[None] & not_pad
    bias = bias_table[_t5_bucket(delta)].astype(jnp.float32)
    bias = bias.transpose(2, 0, 1).reshape(A_KV_HEADS, g_, BLOCK, 2 * BLOCK)
    logits = jnp.where(mask[None, :, None, None], logits + bias[None, None], -jnp.inf)
    sink = sinks.astype(jnp.float32).reshape(A_KV_HEADS, g_)[None, None, :, :, None, None]
    sink = jnp.broadcast_to(sink, logits.shape[:-1] + (1,))
    p = jax.nn.softmax(jnp.concatenate([logits, sink], axis=-1), axis=-1)[..., :-1]
    o = jnp.einsum('bnhgqk,bnkhd->bnqhgd', p.astype(v.dtype), vv)
    return o.reshape(b_, s_, h_, dh)


def _dsa_attention(q, k, v, iq, ik, iw, bias_table):
    b_, s_, h_, dh = q.shape
    g_ = h_ // B_KV_HEADS
    nb = s_ // BLOCK
    topk = min(TOPK_MAX, s_ // 4)
    kpos = jnp.arange(s_)

    def one_block(args):
        n, qb, iqb, iwb = args
        tpos = n * BLOCK + jnp.arange(BLOCK)
        causal = kpos[None, :] <= tpos[:, None]
        sc = jnp.einsum('bqhc,bsc->bqhs', iqb, ik).astype(jnp.float32) * (IDX_DIM ** -0.5)
        w = iwb.astype(jnp.float32) * (IDX_HEADS ** -0.5)
        score = jnp.einsum('bqh,bqhs->bqs', w, jax.nn.relu(sc))
        score = jnp.where(causal[None], score, -jnp.inf)
        _, idx = lax.top_k(score, topk)
        ksel = jax.vmap(lambda kb_, ib: kb_[ib])(k, idx)
        vsel = jax.vmap(lambda vb_, ib: vb_[ib])(v, idx)
        qg = qb.reshape(b_, BLOCK, B_KV_HEADS, g_, dh)
        logits = jnp.einsum('bqhgd,bqkhd->bqhgk', qg, ksel).astype(jnp.float32) * ATTN_SCALE
        delta = tpos[None, :, None] - idx
        bias = bias_table[_t5_bucket(delta)].astype(jnp.float32)
        bias = bias.reshape(b_, BLOCK, topk, B_KV_HEADS, g_).transpose(0, 1, 3, 4, 2)
        valid = (delta >= 0)[:, :, None, None, :]
        logits = jnp.where(valid, logits + bias, -jnp.inf)
        p = jax.nn.softmax(logits, axis=-1).astype(v.dtype)
        o = jnp.einsum('bqhgk,bqkhd->bqhgd', p, vsel)
        return o.reshape(b_, BLOCK, h_, dh)

    out = lax.map(one_block, (jnp.arange(nb), _to_blocks(q, nb), _to_blocks(iq, nb), _to_blocks(iw, nb)))
    return out.swapaxes(0, 1).reshape(b_, s_, h_, dh)


def _forgetting_attention(q, k, v, log_f):
    b_, s_, h_, dh = q.shape
    nb = s_ // BLOCK
    c = jnp.cumsum(log_f, axis=1)
    c_keys = c.transpose(0, 2, 1)
    kpos = jnp.arange(s_)

    def one_block(args):
        n, qb, cb = args
        tpos = n * BLOCK + jnp.arange(BLOCK)
        logits = jnp.einsum('bqhd,bshd->bhqs', qb, k).astype(jnp.float32) * ATTN_SCALE
        logits = logits + cb.transpose(0, 2, 1)[..., None] - c_keys[:, :, None, :]
        causal = kpos[None, :] <= tpos[:, None]
        logits = jnp.where(causal[None, None], logits, -jnp.inf)
        p = jax.nn.softmax(logits, axis=-1).astype(v.dtype)
        return jnp.einsum('bhqs,bshd->bqhd', p, v)

    out = lax.map(one_block, (jnp.arange(nb), _to_blocks(q, nb), _to_blocks(c, nb)))
    return out.swapaxes(0, 1).reshape(b_, s_, h_, dh)


def setup_inputs(seed: int = 0) -> dict:
    key = jax.random.key(seed)
    ks = jax.random.split(key, 9)
    x = jax.random.normal(ks[0], (BATCH, SEQ, D_MODEL), jnp.float32)
    norm_gain = 1.0 + 0.02 * jax.random.normal(ks[1], (DEPTH, D_MODEL), jnp.float32)
    w_in = jax.random.normal(ks[2], (DEPTH, D_MODEL, IN_COLS), jnp.float32) * D_MODEL ** -0.5
    b_forget = FORGET_BIAS_INIT + 0.1 * jax.random.normal(ks[3], (DEPTH, C_HEADS), jnp.float32)
    qk_gain = 1.0 + 0.02 * jax.random.normal(ks[4], (DEPTH, N_BRANCH, 2, HEAD_DIM), jnp.float32)
    sinks = 0.5 * jax.random.normal(ks[5], (DEPTH, A_HEADS), jnp.float32)
    w_branch = jax.random.normal(ks[6], (DEPTH, N_BRANCH, BRANCH_WIDTH, D_MODEL), jnp.float32) * BRANCH_WIDTH ** -0.5
    w_out = jax.random.normal(ks[7], (DEPTH, D_MODEL, D_MODEL), jnp.float32) * D_MODEL ** -0.5
    rel_bias = 0.1 * jax.random.normal(ks[8], (N_BUCKETS, A_HEADS + B_HEADS), jnp.float32)
    return {"x": x, "norm_gain": norm_gain, "w_in": w_in, "b_forget": b_forget,
            "qk_gain": qk_gain, "sinks": sinks, "w_branch": w_branch, "w_out": w_out,
            "rel_bias": rel_bias}


def reference(x, norm_gain, w_in, b_forget, qk_gain, sinks, w_branch, w_out, rel_bias):
    b_, s_, _ = x.shape
    bias_a = rel_bias[:, :A_HEADS]
    bias_b = rel_bias[:, A_HEADS:]
    for layer in range(DEPTH):
        h = _rms_norm(x, norm_gain[layer])
        z = jnp.einsum('bsd,dc->bsc', h, w_in[layer])
        (a_q, a_k, a_v, a_gate,
         b_q, b_k, b_v, b_iq, b_ik, b_iw, b_gate,
         c_q, c_k, c_v, c_f, c_gate,
         merge) = _split(z, IN_SIZES)
        g = qk_gain[layer]

        def heads(t, n):
            return t.reshape(b_, s_, n, HEAD_DIM)

        qa = _rms_norm(heads(a_q, A_HEADS), g[0, 0])
        ka = _rms_norm(heads(a_k, A_KV_HEADS), g[0, 1])
        oa = _sliding_window_attention(qa, ka, heads(a_v, A_KV_HEADS), sinks[layer], bias_a)
        qb = _rms_norm(heads(b_q, B_HEADS), g[1, 0])
        kb = _rms_norm(heads(b_k, B_KV_HEADS), g[1, 1])
        ob = _dsa_attention(qb, kb, heads(b_v, B_KV_HEADS),
                            b_iq.reshape(b_, s_, IDX_HEADS, IDX_DIM), b_ik, b_iw, bias_b)
        qc = _rms_norm(heads(c_q, C_HEADS), g[2, 0])
        kc = _rms_norm(heads(c_k, C_HEADS), g[2, 1])
        log_f = jax.nn.log_sigmoid((c_f + b_forget[layer]).astype(jnp.float32))
        oc = _forgetting_attention(qc, kc, heads(c_v, C_HEADS), log_f)

        branches = jnp.stack([oa.reshape(b_, s_, BRANCH_WIDTH) * jax.nn.silu(a_gate),
                              ob.reshape(b_, s_, BRANCH_WIDTH) * jax.nn.silu(b_gate),
                              oc.reshape(b_, s_, BRANCH_WIDTH) * jax.nn.silu(c_gate)], axis=2)
        y = jnp.einsum('bsnc,ncd->bsnd', branches, w_branch[layer])
        gates = jax.nn.sigmoid(merge.reshape(b_, s_, N_BRANCH, D_MODEL))
        merged = jnp.sum(gates * y, axis=2)
        x = x + jnp.einsum('bsd,de->bse', merged, w_out[layer])
    return x
```

```python
import math
import numpy as np
import ml_dtypes
from contextlib import ExitStack
import concourse.bass as bass
import concourse.mybir as mybir
from concourse.bass_utils import run_bass_kernel_spmd

F32 = mybir.dt.float32
BF16 = mybir.dt.bfloat16
AF = mybir.ActivationFunctionType
ALU = mybir.AluOpType

D = 1024
INC = 7852
EPS = 1e-6
NEGM = -3750.0
TOPK = 256


class Buf:
    __slots__ = ("name", "t", "w", "r", "ds")

    def __init__(self, name, t=None):
        self.name, self.t, self.w, self.r, self.ds = name, t, {}, {}, {}


class Sched:
    def __init__(self, nc, es):
        self.nc, self.es = nc, es
        self.eng = {"pe": nc.tensor, "act": nc.scalar, "dve": nc.vector, "pool": nc.gpsimd, "sp": nc.sync}
        self.psem = {k: es.enter_context(nc.semaphore("p_" + k)) for k in ("pe", "act", "dve", "pool")}
        self.pcnt = {k: 0 for k in self.psem}
        self.waited = {k: {} for k in self.eng}
        self.bufs = []
        self.ninst = 0
        self.sempool = {"sp": [], "pool": []}
        self.nsem = 4

    def buf(self, name, t=None):
        b = Buf(name, t)
        self.bufs.append(b)
        return b

    def _wait(self, e, deps):
        for sid, (sem, val) in deps.items():
            if val <= 0 or self.waited[e].get(sid, 0) >= val:
                continue
            if e == "pe" and sem is self.psem["pe"]:
                continue
            self.eng[e].wait_ge(sem, val)
            self.waited[e][sid] = val
            self.ninst += 1

    @staticmethod
    def _merge(deps, d):
        for sid, tok in d.items():
            if sid not in deps or deps[sid][1] < tok[1]:
                deps[sid] = tok

    def _deps(self, reads, writes, awrites):
        deps = {}
        for b in reads:
            self._merge(deps, b.w)
        for b in writes:
            self._merge(deps, b.w)
            self._merge(deps, b.r)
        for b in awrites:
            self._merge(deps, b.r)
        return deps

    def _post(self, sem, val, reads, writes, awrites):
        tok = (sem, val)
        for b in reads:
            b.r[id(sem)] = tok
        for b in writes:
            b.w = {id(sem): tok}
            b.r = {}
        for b in awrites:
            b.w[id(sem)] = tok

    def op(self, e, fn, reads=(), writes=(), awrites=()):
        self._wait(e, self._deps(reads, writes, awrites))
        inst = fn(self.eng[e])
        self.pcnt[e] += 1
        inst.then_inc(self.psem[e], 1)
        self.ninst += 1
        self._post(self.psem[e], self.pcnt[e], reads, writes, awrites)
        return inst

    def dma(self, q, out, in_, owner, reads=(), writes=(), awrites=()):
        if q not in owner.ds:
            if self.sempool[q]:
                owner.ds[q] = list(self.sempool[q].pop())
            else:
                owner.ds[q] = [self.es.enter_context(self.nc.semaphore("d%s_%s" % (q, owner.name))), 0]
                self.nsem += 1
        d = owner.ds[q]
        deps = self._deps(reads, writes, awrites)
        self._merge(deps, {id(d[0]): (d[0], d[1])})
        self._wait(q, deps)
        inst = self.eng[q].dma_start(out=out, in_=in_)
        d[1] += 16
        inst.then_inc(d[0], 16)
        self.ninst += 1
        self._post(d[0], d[1], reads, writes, awrites)
        return inst

    def release(self, b):
        for q, d in b.ds.items():
            self.sempool[q].append((d[0], d[1]))
        b.ds = {}
        if b in self.bufs:
            self.bufs.remove(b)

    def barrier(self):
        toks = {id(s): (s, self.pcnt[k]) for k, s in self.psem.items()}
        for b in self.bufs:
            for d in b.ds.values():
                toks[id(d[0])] = (d[0], d[1])
        for e in self.eng:
            self._wait(e, toks)
        for b in self.bufs:
            b.w, b.r = {}, {}


def _pois_tail(m, k):
    p = math.exp(-m)
    c = p
    for i in range(1, k + 1):
        p *= m / i
        c += p
    return max(0.0, 1.0 - c)


def _topk_plan(n):
    best = (64 * n if n <= 1024 else 10**12, None)
    for cs in (128, 256, 512, 1024):
        nchunk = (n + cs - 1) // cs
        if nchunk < 2:
            continue
        m = TOPK * cs / n
        for r in range(1, 17):
            if 8 * r > cs or 8 * r * nchunk < TOPK + 16:
                continue
            if _pois_tail(m, 8 * r) * nchunk < 1e-10:
                cost = (2 * r - 1) * n + 66 * nchunk * 8 * r + 150 * nchunk * (2 * r - 1)
                if cost < best[0]:
                    best = (cost, (cs, r))
                break
    if False:
        cand = None
        for cs in (128, 256, 512, 1024):
            nchunk = (n + cs - 1) // cs
            m = TOPK * cs / n
            for r in range(1, 17):
                if 8 * r > cs or 8 * r * nchunk < TOPK + 16:
                    continue
                if _pois_tail(m, 8 * r) * nchunk < 1e-10:
                    cost = (2 * r - 1) * n + 66 * nchunk * 8 * r
                    if cand is None or cost < cand[0]:
                        cand = (cost, (cs, r))
                    break
        return cand[1]
    return best[1]


def build_program(S, depth=2, dbg=False):
    NG = S // 512
    NGP = NG + 1
    SP = NGP * 512
    NT = NGP * 4
    own_by_layer = [list(range(1, NG + 1)) if l < depth - 1 else list(range(2, NG + 1, 2)) for l in range(depth)]
    TOKQ = NG * 512
    NOUT = len(own_by_layer[-1])

    nc = bass.Bass("TRN2", target_bir_lowering=False)
    dt_in = lambda n, s, d=F32: nc.dram_tensor(n, list(s), d, kind="ExternalInput").ap()
    dt_sc = lambda n, s, d=BF16: nc.dram_tensor(n, list(s), d, kind="Internal").ap()
    xs = dt_in("xs", [SP, D])
    w_in = dt_in("w_in", [depth, D, INC])
    w_br = dt_in("w_br", [depth, 3, 512, D])
    w_out = dt_in("w_out", [depth, D, D])
    gainbc = dt_in("gainbc", [depth, 128, D])
    qkg = dt_in("qkg", [depth, 128, 8])
    bfc = dt_in("bfc", [depth, 128, 1])
    sinkrep = dt_in("sinkrep", [depth, 128, 1024])
    t5a = dt_in("t5a", [2, 128, 1024])
    t5b = dt_in("t5b", [6, 128, 1024])
    NC32 = 128 * 4 + 64 + 128 + NT + 512
    c32 = dt_in("c32", [128, NC32])
    kbrow = dt_in("kbrow", [1, SP])
    NCBF = 128 * 5 + 4 * 512
    cbf = dt_in("cbf", [128, NCBF], BF16)
    y = nc.dram_tensor("y", [NOUT * 512, D], F32, kind="ExternalOutput").ap()

    QT = dt_sc("QT", [1664, TOKQ])
    KT = dt_sc("KT", [800, SP])
    VV = dt_sc("VV", [SP, 896])
    GT = dt_sc("GT", [1536, TOKQ])
    MT = dt_sc("MT", [3072, TOKQ])
    BR = dt_sc("BR", [1536, TOKQ])
    X1 = dt_sc("X1", [SP, D], F32)
    dbgo = {}

    es = ExitStack()
    with es:
        sc = Sched(nc, es)

        uid = [0]

        def sb(st, name, shape, dt=F32):
            uid[0] += 1
            name = "%s_%d" % (name, uid[0])
            b = sc.buf(name, st.enter_context(nc.sbuf_tensor(name, list(shape), dt)))
            if st is not es:
                st.callback(sc.release, b)
            return b

        def pst(st, name, shape, dt=F32):
            return sc.buf(name, st.enter_context(nc.psum_tensor(name, list(shape), dt)))

        dQT, dKT, dVV, dGT, dMT, dBR, dX1, dY = (sc.buf(n) for n in ("dQT", "dKT", "dVV", "dGT", "dMT", "dBR", "dX1", "dY"))

        C32 = sb(es, "C32", [128, NC32])
        CBF = sb(es, "CBF", [128, NCBF], BF16)
        o = 0
        IDF = C32.t[:, 0:128]; ONESF = C32.t[:, 128:256]; BDF = C32.t[:, 256:384]; E0F = C32.t[:, 384:512]
        SHF = C32.t[:, 512:576]; CAUS = C32.t[:, 576:704]
        KBIAS = C32.t[:, 704:704 + NT]; KPAD = C32.t[:, 704 + NT:704 + NT + 512]
        I8 = CBF.t[:, 0:128]; INEG = CBF.t[:, 128:256]; MA0 = CBF.t[:, 256:384]; MA1 = CBF.t[:, 384:512]
        CM = [CBF.t[:, 512 + 512 * i:512 + 512 * (i + 1)] for i in range(4)]
        IDB = CBF.t[:, 2560:2688]
        ONE8 = sb(es, "ONE8", [8, 512])
        EPSC = sb(es, "EPSC", [128, 1])
        PS = [pst(es, "ps%d" % i, [128, 512]) for i in range(7)]
        PSB = pst(es, "psb", [128, 512])
        PSBv = PSB.t[:, :].bitcast(BF16)

        sc.dma("sp", C32.t[:, :], c32[:, :], C32, writes=[C32])
        sc.dma("sp", CBF.t[:, :], cbf[:, :], CBF, writes=[CBF])
        sc.op("pool", lambda e: e.memset(ONE8.t[:, :], 1.0), writes=[ONE8])
        sc.op("pool", lambda e: e.memset(EPSC.t[:, :], 1e-18), writes=[EPSC])
        sc.barrier()

        def build_bma(st):
            BMA = sb(st, "BMA", [128, 2, 8, 128], BF16)
            tmp = sb(st, "t5tmp", [128, 1024])
            for dd in range(2):
                sc.dma("sp", tmp.t[:, :], t5a[dd], tmp, writes=[tmp])
                msk = MA0 if dd == 0 else MA1
                sc.op("dve", lambda e: e.tensor_tensor(
                    out=BMA.t[:, dd], in0=tmp.t[:, :].rearrange("p (h q) -> p h q", h=8),
                    in1=msk.unsqueeze(1).to_broadcast([128, 8, 128]), op=ALU.add), reads=[tmp, CBF], awrites=[BMA])
            return BMA

        def build_bmb(st):
            BMB = sb(st, "BMB", [128, 5, 8, 128], BF16)
            with ExitStack() as st2:
                tmp = sb(st2, "t5tmpb", [128, 1024])
                tfar = sb(st2, "t5far", [128, 1024])
                sc.dma("sp", tfar.t[:, :], t5b[5], tfar, writes=[tfar])
                for dd in range(5):
                    sc.dma("sp", tmp.t[:, :], t5b[dd], tmp, writes=[tmp])
                    sc.op("dve", lambda e: e.tensor_tensor(out=tmp.t[:, :], in0=tmp.t[:, :], in1=tfar.t[:, :], op=ALU.subtract),
                          reads=[tfar, tmp], writes=[tmp])
                    if dd == 0:
                        sc.op("dve", lambda e: e.tensor_tensor(
                            out=BMB.t[:, 0], in0=tmp.t[:, :].rearrange("p (h q) -> p h q", h=8),
                            in1=MA0.unsqueeze(1).to_broadcast([128, 8, 128]), op=ALU.add), reads=[tmp, CBF], awrites=[BMB])
                    else:
                        sc.op("dve", lambda e: e.tensor_copy(
                            out=BMB.t[:, dd], in_=tmp.t[:, :].rearrange("p (h q) -> p h q", h=8)), reads=[tmp], awrites=[BMB])
                sc.barrier()
            return BMB

        def attn_epilogue(acc, T1, dps, tmps, gate_ap, gate_buf, out_ap, out_buf, sink_ap=None, sink_buf=None):
            d2, ln, rd, t2 = tmps
            sc.op("act", lambda e: e.activation(out=T1.t[:, :], in_=acc.t[:, :], func=AF.Copy), reads=[acc], writes=[T1])
            sc.op("pe", lambda e: e.matmul(dps.t[:64, :], lhsT=SHF, rhs=T1.t[:, :], start=True, stop=True),
                  reads=[T1, C32], writes=[dps])
            if sink_ap is not None:
                sc.op("dve", lambda e: e.tensor_tensor(out=d2.t[:64, :], in0=dps.t[:64, :], in1=sink_ap, op=ALU.add),
                      reads=[dps, sink_buf], writes=[d2])
                sc.op("act", lambda e: e.activation(out=ln.t[:64, :], in_=d2.t[:64, :], func=AF.Ln), reads=[d2], writes=[ln])
            else:
                sc.op("act", lambda e: e.activation(out=ln.t[:64, :], in_=dps.t[:64, :], func=AF.Ln, bias=EPSC.t[:64, 0:1]),
                      reads=[dps, EPSC], writes=[ln])
            sc.op("act", lambda e: e.activation(out=rd.t[:64, :], in_=ln.t[:64, :], func=AF.Exp, scale=-1.0),
                  reads=[ln], writes=[rd])
            sc.op("pool", lambda e: e.tensor_tensor(out=t2.t[:64, :], in0=T1.t[:64, :], in1=rd.t[:64, :], op=ALU.mult),
                  reads=[T1, rd], writes=[t2])
            sc.op("pool", lambda e: e.tensor_tensor(out=out_ap, in0=t2.t[:64, :], in1=gate_ap, op=ALU.mult),
                  reads=[t2, gate_buf], awrites=[out_buf])

        for l in range(depth):
            own = own_by_layer[l]
            nown = len(own)
            xsrc, dXS = (xs, None) if l == 0 else (X1, dX1)
            xdst, dXD = (X1, dX1) if l < depth - 1 else (y, dY)
            lst = ExitStack()
            with lst:
                ckey = sb(lst, "ckey", [128, NT, 8])
                crefbc = sb(lst, "crefbc", [128, NGP, 8])
                iwall = sb(lst, "iwall", [128, nown * 4, 4])
                qkgs = sb(lst, "qkgs", [128, 8])
                sc.dma("sp", qkgs.t[:, :], qkg[l], qkgs, writes=[qkgs])

                for (cbase, ccount, pname) in [(0, 4780, "main"), (4780, 3072, "merge")]:
                  with ExitStack() as st:
                    Wbf = sb(st, "Wbf", [128, 8, ccount], BF16)
                    wst = [sb(st, "wst%d" % i, [128, 8, 128]) for i in range(2)]
                    gbc = sb(st, "gbc", [128, D])
                    bfs = sb(st, "bfs", [128, 1])
                    xts = [sb(st, "xt%d" % i, [128, D]) for i in range(2)]
                    junk = sb(st, "junk", [128, D], BF16)
                    hb = [sb(st, "hb%d" % i, [128, D], BF16) for i in range(2)]
                    hT = [sb(st, "hT%d" % i, [128, 8, 512], BF16) for i in range(2)]
                    ss = [sb(st, "ss%d" % i, [128, 4]) for i in range(2)]
                    sq = sb(st, "sq", [128, 512]); lnb = sb(st, "lnb", [128, 512]); rsb = sb(st, "rsb", [128, 512])
                    stg = [sb(st, "stg%d" % i, [128, 512], BF16) for i in range(4)]
                    vst = [sb(st, "vst%d" % i, [128, 1024], BF16) for i in range(2)]
                    fz = sb(st, "fz", [8, 512]); fe = sb(st, "fe", [8, 512]); fsp = sb(st, "fsp", [8, 512])
                    cg = sb(st, "cg", [8, 512]); cprev = sb(st, "cprev", [8, 1])
                    sc.dma("sp", gbc.t[:, :], gainbc[l], gbc, writes=[gbc])
                    sc.dma("sp", bfs.t[:, :], bfc[l], bfs, writes=[bfs])
                    sc.op("dve", lambda e: e.memset(cprev.t[:, :], 0.0), writes=[cprev])
                    for v_ in vst:
                        sc.op("pool", lambda e: e.memset(v_.t[:, :], 1.0), writes=[v_])
                    wv = w_in[l].rearrange("(c p) n -> p c n", p=128)
                    ceng = ["dve", "pool", "act"]
                    for ci, c0 in enumerate(range(0, ccount, 128)):
                        cw = min(128, ccount - c0)
                        s_ = wst[ci % 2]
                        sc.dma("sp", s_.t[:, :, :cw], wv[:, :, cbase + c0:cbase + c0 + cw], s_, writes=[s_])
                        en = ceng[ci % 3]
                        if en == "act":
                            sc.op("act", lambda e: e.activation(out=Wbf.t[:, :, c0:c0 + cw], in_=s_.t[:, :, :cw], func=AF.Copy),
                                  reads=[s_], awrites=[Wbf])
                        else:
                            sc.op(en, lambda e: e.tensor_copy(out=Wbf.t[:, :, c0:c0 + cw], in_=s_.t[:, :, :cw]),
                                  reads=[s_], awrites=[Wbf])
                    stg_i = [0]
                    psr = [0]

                    def nextps():
                        psr[0] = (psr[0] + 1) % 4
                        return PS[psr[0]]

                    def store_fm(src_ap_fn, M, dram_ap, dbuf):
                        s_ = stg[stg_i[0] % 4]; stg_i[0] += 1
                        src_ap_fn(s_)
                        sc.dma("pool", dram_ap, s_.t[:M, :], s_, reads=[s_], awrites=[dbuf])

                    def fm_mm(ps, c0, M, h):
                        c0 = c0 - cbase
                        for c in range(8):
                            sc.op("pe", lambda e: e.matmul(ps.t[:M, :], lhsT=Wbf.t[:, c, c0:c0 + M], rhs=h.t[:, c, :],
                                                           start=(c == 0), stop=(c == 7)), reads=[Wbf, h], writes=[ps])

                    def ep_norm(ps, gcol, dram_ap, dbuf):
                        sc.op("act", lambda e: e.activation(out=sq.t[:, :], in_=ps.t[:, :], func=AF.Square), reads=[ps], writes=[sq])
                        sc.op("pe", lambda e: e.matmul(PS[4].t[:, :], lhsT=BDF, rhs=sq.t[:, :], start=True, stop=True),
                              reads=[sq, C32], writes=[PS[4]])
                        sc.op("dve", lambda e: e.tensor_scalar(out=lnb.t[:, :], in0=PS[4].t[:, :], scalar1=EPS, scalar2=None, op0=ALU.add),
                              reads=[PS[4]], writes=[lnb])
                        sc.op("act", lambda e: e.activation(out=rsb.t[:, :], in_=lnb.t[:, :], func=AF.Ln), reads=[lnb], writes=[rsb])
                        sc.op("act", lambda e: e.activation(out=lnb.t[:, :], in_=rsb.t[:, :], func=AF.Exp, scale=-0.5), reads=[rsb], writes=[lnb])
                        store_fm(lambda s_: sc.op("dve", lambda e: e.scalar_tensor_tensor(
                            out=s_.t[:, :], in0=ps.t[:, :], scalar=qkgs.t[:, gcol:gcol + 1], in1=lnb.t[:, :], op0=ALU.mult, op1=ALU.mult),
                            reads=[ps, qkgs, lnb], writes=[s_]), 128, dram_ap, dbuf)

                    def ep_act(ps, M, func, dram_ap, dbuf):
                        store_fm(lambda s_: sc.op("act", lambda e: e.activation(out=s_.t[:M, :], in_=ps.t[:M, :], func=func),
                                                  reads=[ps], writes=[s_]), M, dram_ap, dbuf)

                    for g in range(1, NG + 1):
                        h = hT[g % 2]
                        isown = g in own
                        si = own.index(g) if isown else -1
                        if pname == "merge" and not isown:
                            continue
                        for tt in range(4):
                            tile = 4 * g + tt
                            xt = xts[tt % 2]; hbb = hb[tt % 2]; s4 = ss[tt % 2]
                            sc.dma("sp", xt.t[:, :], xsrc[tile * 128:(tile + 1) * 128, :], xt, reads=([dXS] if dXS else []), writes=[xt])
                            sc.op("act", lambda e: e.activation(out=junk.t[:, :], in_=xt.t[:, :], func=AF.Square, accum_out=s4.t[:, 0:1]),
                                  reads=[xt], writes=[junk, s4])
                            sc.op("dve", lambda e: e.tensor_scalar(out=s4.t[:, 1:2], in0=s4.t[:, 0:1], scalar1=1.0 / D, scalar2=EPS,
                                                                    op0=ALU.mult, op1=ALU.add), reads=[s4], writes=[s4])
                            sc.op("act", lambda e: e.activation(out=s4.t[:, 2:3], in_=s4.t[:, 1:2], func=AF.Ln), reads=[s4], writes=[s4])
                            sc.op("act", lambda e: e.activation(out=s4.t[:, 3:4], in_=s4.t[:, 2:3], func=AF.Exp, scale=-0.5), reads=[s4], writes=[s4])
                            sc.op("dve", lambda e: e.scalar_tensor_tensor(out=hbb.t[:, :], in0=xt.t[:, :], scalar=s4.t[:, 3:4], in1=gbc.t[:, :],
                                                                           op0=ALU.mult, op1=ALU.mult), reads=[xt, s4, gbc], writes=[hbb])
                            for c in range(8):
                                sc.op("pe", lambda e: e.transpose(out=PSBv[:, c * 128:(c + 1) * 128], in_=hbb.t[:, c * 128:(c + 1) * 128],
                                                                  identity=IDB), reads=[hbb, CBF], writes=[PSB])
                            sc.op("dve", lambda e: e.tensor_copy(out=h.t[:, :, tt * 128:(tt + 1) * 128],
                                                                 in_=PSBv.rearrange("p (c t) -> p c t", c=8)), reads=[PSB], awrites=[h])
                        tk = slice(g * 512, (g + 1) * 512)
                        if pname == "merge":
                            tq = slice(si * 512, (si + 1) * 512)
                            for i in range(24):
                                ps = nextps(); fm_mm(ps, 4780 + 128 * i, 128, h); ep_act(ps, 128, AF.Sigmoid, MT[128 * i:128 * (i + 1), tq], dMT)
                            continue
                        for (c0, gcol, r0) in [(512, 1, 0), (1792, 3, 128)] + [(3236 + 128 * i, 5, 256 + 128 * i) for i in range(4)]:
                            ps = nextps(); fm_mm(ps, c0, 128, h); ep_norm(ps, gcol, KT[r0:r0 + 128, tk], dKT)
                        ps = nextps(); fm_mm(ps, 2176, 32, h); ep_act(ps, 32, AF.Copy, KT[768:800, tk], dKT)
                        ps = nextps(); fm_mm(ps, 4260, 8, h)
                        sc.op("dve", lambda e: e.tensor_scalar(out=fz.t[:, :], in0=ps.t[:8, :], scalar1=bfs.t[:8, 0:1], scalar2=None, op0=ALU.add),
                              reads=[ps, bfs], writes=[fz])
                        sc.op("act", lambda e: e.activation(out=fe.t[:, :], in_=fz.t[:, :], func=AF.Exp, scale=-1.0), reads=[fz], writes=[fe])
                        sc.op("dve", lambda e: e.tensor_scalar(out=fz.t[:, :], in0=fe.t[:, :], scalar1=1.0, scalar2=None, op0=ALU.add),
                              reads=[fe], writes=[fz])
                        sc.op("act", lambda e: e.activation(out=fsp.t[:, :], in_=fz.t[:, :], func=AF.Ln), reads=[fz], writes=[fsp])
                        sc.op("dve", lambda e: e.tensor_tensor_scan(out=cg.t[:, :], data0=ONE8.t[:, :], data1=fsp.t[:, :], initial=cprev.t[:, 0:1],
                                                                     op0=ALU.mult, op1=ALU.subtract), reads=[ONE8, fsp, cprev], writes=[cg])
                        sc.op("dve", lambda e: e.tensor_copy(out=cprev.t[:, :], in_=cg.t[:, 511:512]), reads=[cg], writes=[cprev])
                        for tt in range(4):
                            sc.op("pe", lambda e: e.transpose(out=PS[5].t[:, tt * 8:(tt + 1) * 8], in_=cg.t[:8, tt * 128:(tt + 1) * 128],
                                                              identity=C32.t[:8, 0:8]), reads=[cg, C32], writes=[PS[5]])
                        sc.op("dve", lambda e: e.tensor_copy(out=ckey.t[:, 4 * g:4 * g + 4, :], in_=PS[5].t[:, 0:32].rearrange("p (t h) -> p t h", t=4)),
                              reads=[PS[5]], awrites=[ckey])
                        sc.op("pe", lambda e: e.matmul(PS[5].t[:, 32:40], lhsT=E0F, rhs=ckey.t[:, 4 * g, :], start=True, stop=True),
                              reads=[ckey, C32], writes=[PS[5]])
                        sc.op("dve", lambda e: e.tensor_copy(out=crefbc.t[:, g, :], in_=PS[5].t[:, 32:40]), reads=[PS[5]], awrites=[crefbc])
                        for tt in range(4):
                            tile = 4 * g + tt
                            vs_ = vst[tt % 2]
                            for (c0, n, pb, o0) in [(640, 128, PS[5], 0), (1920, 128, PS[5], 128), (3748, 512, PS[6], 0)]:
                                for c in range(8):
                                    sc.op("pe", lambda e: e.matmul(pb.t[:, o0:o0 + n], lhsT=h.t[:, c, tt * 128:(tt + 1) * 128],
                                                                   rhs=Wbf.t[:, c, c0 - cbase:c0 - cbase + n], start=(c == 0), stop=(c == 7)),
                                          reads=[Wbf, h], writes=[pb])
                            sc.op("dve", lambda e: e.tensor_copy(out=vs_.t[:, 0:128], in_=PS[5].t[:, 0:128]), reads=[PS[5]], awrites=[vs_])
                            sc.op("dve", lambda e: e.tensor_copy(out=vs_.t[:, 128:384].rearrange("p (h x) -> p h x", h=2)[:, :, 0:64],
                                                                 in_=PS[5].t[:, 128:256].rearrange("p (h d) -> p h d", h=2)), reads=[PS[5]], awrites=[vs_])
                            sc.op("act", lambda e: e.activation(out=vs_.t[:, 384:896], in_=PS[6].t[:, :], func=AF.Copy), reads=[PS[6]], awrites=[vs_])
                            sc.dma("pool", VV[tile * 128:(tile + 1) * 128, :], vs_.t[:, 0:896], vs_, reads=[vs_], awrites=[dVV])
                            if isown:
                                for c in range(8):
                                    sc.op("pe", lambda e: e.matmul(PS[5].t[:, 256:260], lhsT=h.t[:, c, tt * 128:(tt + 1) * 128],
                                                                   rhs=Wbf.t[:, c, 2208 - cbase:2212 - cbase], start=(c == 0), stop=(c == 7)),
                                          reads=[Wbf, h], writes=[PS[5]])
                                sc.op("dve", lambda e: e.tensor_copy(out=iwall.t[:, si * 4 + tt, :], in_=PS[5].t[:, 256:260]),
                                      reads=[PS[5]], awrites=[iwall])
                        if not isown:
                            continue
                        tq = slice(si * 512, (si + 1) * 512)
                        for (b0, gcol, r0) in [(0, 0, 0), (1280, 2, 512), (2724, 4, 1024)]:
                            for i in range(4):
                                ps = nextps(); fm_mm(ps, b0 + 128 * i, 128, h); ep_norm(ps, gcol, QT[r0 + 128 * i:r0 + 128 * (i + 1), tq], dQT)
                        ps = nextps(); fm_mm(ps, 2048, 128, h); ep_act(ps, 128, AF.Copy, QT[1536:1664, tq], dQT)
                        for (b0, r0) in [(768, 0), (2212, 512), (4268, 1024)]:
                            for i in range(4):
                                ps = nextps(); fm_mm(ps, b0 + 128 * i, 128, h); ep_act(ps, 128, AF.Silu, GT[r0 + 128 * i:r0 + 128 * (i + 1), tq], dGT)
                    sc.barrier()

                with ExitStack() as st:
                    BMA = build_bma(st)
                    sinkx = sb(st, "sinkx", [128, 1024])
                    sc.dma("sp", sinkx.t[:, :], sinkrep[l], sinkx, writes=[sinkx])
                    sc.op("act", lambda e: e.activation(out=sinkx.t[:, :], in_=sinkx.t[:, :], func=AF.Exp), reads=[sinkx], writes=[sinkx])
                    qa = [sb(st, "qa%d" % i, [64, 8, 512], BF16) for i in range(2)]
                    ga = [sb(st, "ga%d" % i, [64, 8, 512], BF16) for i in range(2)]
                    ka = [sb(st, "ka%d" % i, [64, 2, 640], BF16) for i in range(2)]
                    va = [sb(st, "va%d" % i, [128, 5, 2, 128], BF16) for i in range(2)]
                    oa = [sb(st, "oa%d" % i, [64, 8, 512], BF16) for i in range(2)]
                    pt = [sb(st, "pt%d" % i, [128, 512], BF16) for i in range(3)]
                    T1 = sb(st, "T1", [128, 512]); tmps = [sb(st, "ta%d" % i, [64, 512]) for i in range(4)]
                    for v_ in va:
                        sc.op("pool", lambda e, v_=v_: e.memset(v_.t[:, :, :, 64:128], 1.0), awrites=[v_])
                    pti = 0
                    for si, g in enumerate(own):
                        q_, g_, k_, v_, o_ = qa[si % 2], ga[si % 2], ka[si % 2], va[si % 2], oa[si % 2]
                        tq = slice(si * 512, (si + 1) * 512)
                        sc.dma("sp", q_.t[:, :, :], QT[0:512, tq].rearrange("(h d) t -> d h t", d=64), q_, reads=[dQT], writes=[q_])
                        sc.dma("sp", g_.t[:, :, :], GT[0:512, tq].rearrange("(h d) t -> d h t", d=64), g_, reads=[dGT], writes=[g_])
                        t0 = 4 * g - 1
                        f0 = 1 if g == 1 else 0
                        sc.dma("sp", k_.t[:, :, f0 * 128:], KT[0:128, (t0 + f0) * 128:(t0 + 5) * 128].rearrange("(h d) t -> d h t", d=64), k_,
                               reads=[dKT], writes=[k_])
                        for kv in range(2):
                            sc.dma("sp", v_.t[:, f0:, kv, 0:64], VV[(t0 + f0) * 128:(t0 + 5) * 128, 64 * kv:64 * (kv + 1)].rearrange("(t p) d -> p t d", p=128), v_,
                                   reads=[dVV], awrites=[v_])
                        for kv in range(2):
                            for ql in range(4):
                                acc = PS[4 + (ql % 2)]
                                kts = [ql, ql + 1] if not (g == 1 and ql == 0) else [ql + 1]
                                for n_, ktl in enumerate(kts):
                                    ps = PS[pti % 3]; p_ = pt[pti % 3]; pti += 1
                                    dd = 1 if ktl == ql else 0
                                    sc.op("pe", lambda e: e.matmul(ps.t[:, :].rearrange("p (h q) -> p h q", h=4), lhsT=k_.t[:, kv, ktl * 128:(ktl + 1) * 128],
                                                                   rhs=q_.t[:, 4 * kv:4 * kv + 4, ql * 128:(ql + 1) * 128], start=True, stop=False),
                                          reads=[k_, q_], writes=[ps])
                                    sc.op("pe", lambda e: e.matmul(ps.t[:, :].rearrange("p (h q) -> p h q", h=4), lhsT=I8,
                                                                   rhs=BMA.t[:, dd, 4 * kv:4 * kv + 4, :], start=False, stop=True),
                                          reads=[CBF, BMA], writes=[ps])
                                    kt_abs = t0 + ktl
                                    sc.op("act", lambda e: e.activation(out=p_.t[:, :], in_=ps.t[:, :], func=AF.Exp, scale=0.125,
                                                                        bias=KBIAS[:, kt_abs:kt_abs + 1]), reads=[ps, C32], writes=[p_])
                                    sc.op("pe", lambda e: e.matmul(acc.t[:, :], lhsT=v_.t[:, ktl, kv, :], rhs=p_.t[:, :],
                                                                   start=(n_ == 0), stop=(n_ == len(kts) - 1)), reads=[v_, p_], writes=[acc])
                                attn_epilogue(acc, T1, PS[6], tmps,
                                              g_.t[:, 4 * kv:4 * kv + 4, ql * 128:(ql + 1) * 128],
                                              g_, o_.t[:, 4 * kv:4 * kv + 4, ql * 128:(ql + 1) * 128], o_,
                                              sink_ap=sinkx.t[:64, kv * 512:(kv + 1) * 512], sink_buf=sinkx)
                        sc.dma("pool", BR[0:512, tq].rearrange("(h d) t -> d h t", d=64), o_.t[:, :, :], o_, reads=[o_], awrites=[dBR])
                    sc.barrier()

                with ExitStack() as st:
                    BMB = build_bmb(st)
                    NKEY = (NT - 4) * 128
                    SCR = sb(st, "SCR", [128, NKEY])
                    NS = sb(st, "NS", [128, NT, 128], BF16)
                    kb = sb(st, "kb", [128, 2, SP], BF16)
                    vbc = [sb(st, "vbc%d" % i, [128, 4, 256], BF16) for i in range(4)]
                    iq = [sb(st, "iq%d" % i, [128, 4, 128], BF16) for i in range(2)]
                    qb = [sb(st, "qb%d" % i, [64, 8, 128], BF16) for i in range(2)]
                    gb = [sb(st, "gb%d" % i, [64, 8, 128], BF16) for i in range(2)]
                    ob = [sb(st, "ob%d" % i, [64, 8, 128], BF16) for i in range(2)]
                    rr = [sb(st, "rr%d" % i, [128, 512]) for i in range(8)]
                    kbc = [sb(st, "kbc%d" % i, [128, 512]) for i in range(4)]
                    kbi = [0]
                    dw = sb(st, "dw", [128, 4, 128])
                    pt = [sb(st, "ptb%d" % i, [128, 512], BF16) for i in range(4)]
                    T1 = sb(st, "T1b", [128, 512]); tmps = [sb(st, "tb%d" % i, [64, 512]) for i in range(4)]
                    NCAND = 1152
                    cand = [sb(st, "cand%d" % i, [128, NCAND]) for i in range(2)]
                    wk = [sb(st, "wk%d" % i, [128, 1024]) for i in range(2)]
                    m8 = sb(st, "m8", [128, 8 * 34])
                    thr = sb(st, "thr", [128, 2]); dthr = sb(st, "dthr", [128, 128]); thrbc = sb(st, "thrbc", [128, 128])
                    sc.dma("sp", kb.t[:64, :, 512:], KT[128:256, 512:].rearrange("(h d) t -> d h t", d=64), kb, reads=[dKT], awrites=[kb])
                    sc.dma("sp", kb.t[64:96, 0, 512:], KT[768:800, 512:], kb, reads=[dKT], awrites=[kb])
                    sc.barrier()

                    kch = [sb(st, "kch%d" % i, [64, 512], BF16) for i in range(4)]
                    vch = [sb(st, "vch%d" % i, [128, 4, 128], BF16) for i in range(4)]
                    qc = [sb(st, "qc%d" % i, [64, 512], BF16) for i in range(2)]
                    gc = [sb(st, "gc%d" % i, [64, 512], BF16) for i in range(2)]
                    oc = [sb(st, "oc%d" % i, [64, 512], BF16) for i in range(2)]
                    bias_h = [sb(st, "biash%d" % i, [128, NT]) for i in range(2)]
                    ptc = [sb(st, "ptc%d" % i, [128, 512], BF16) for i in range(3)]
                    for v_ in vch:
                        sc.op("pool", lambda e: e.memset(v_.t[:, :, 64:128], 1.0), awrites=[v_])

                    def c_gen():
                        it = 0
                        cci = 0
                        psi = 0
                        for hh in range(8):
                            for si, g in enumerate(own):
                                q_, g_, o_, bh = qc[it % 2], gc[it % 2], oc[it % 2], bias_h[it % 2]
                                acc = PSB
                                it += 1
                                tq = slice(si * 512, (si + 1) * 512)
                                sc.dma("sp", q_.t[:, :], QT[1024 + 64 * hh:1024 + 64 * (hh + 1), tq], q_, reads=[dQT], writes=[q_])
                                sc.dma("sp", g_.t[:, :], GT[1024 + 64 * hh:1024 + 64 * (hh + 1), tq], g_, reads=[dGT], writes=[g_])
                                nk = 4 * g + 4
                                sc.op("dve", lambda e: e.tensor_scalar(out=bh.t[:, 4:nk], in0=ckey.t[:, 4:nk, hh], scalar1=crefbc.t[:, g, hh:hh + 1],
                                                                        scalar2=-1.0, op0=ALU.subtract, op1=ALU.mult), reads=[ckey, crefbc], writes=[bh])
                                sc.op("dve", lambda e: e.tensor_tensor(out=bh.t[:, 4:nk], in0=bh.t[:, 4:nk], in1=KBIAS[:, 4:nk], op=ALU.add),
                                      reads=[bh, C32], writes=[bh])
                                kts = list(range(4, nk))
                                chunk = {}

                                def load(k0):
                                    nonlocal cci
                                    k_, v_ = kch[cci % 4], vch[cci % 4]
                                    cci += 1
                                    sc.dma("sp", k_.t[:, :], KT[256 + 64 * hh:256 + 64 * (hh + 1), k0 * 128:(k0 + 4) * 128], k_, reads=[dKT], writes=[k_])
                                    sc.dma("sp", v_.t[:, :, 0:64], VV[k0 * 128:(k0 + 4) * 128, 384 + 64 * hh:384 + 64 * (hh + 1)].rearrange("(t p) d -> p t d", p=128),
                                           v_, reads=[dVV], awrites=[v_])
                                    chunk[k0] = (k_, v_)

                                def qk(kt, pidx):
                                    ps = PS[2 + pidx % 2]
                                    k0 = 4 + 4 * ((kt - 4) // 4)
                                    if k0 not in chunk:
                                        load(k0)
                                    k_ = chunk[k0][0]
                                    diag = kt >= 4 * g
                                    sc.op("pe", lambda e: e.matmul(ps.t[:, :], lhsT=k_.t[:, (kt - k0) * 128:(kt - k0 + 1) * 128], rhs=q_.t[:, :],
                                                                   start=True, stop=not diag), reads=[k_, q_], writes=[ps])
                                    if diag:
                                        sc.op("pe", lambda e: e.matmul(ps.t[:, :], lhsT=I8, rhs=CM[kt - 4 * g], start=False, stop=True),
                                              reads=[CBF], writes=[ps])
                                load(4)
                                qk(kts[0], psi)
                                for n_, kt in enumerate(kts):
                                    if n_ + 1 < len(kts):
                                        k0n = 4 + 4 * ((kts[n_ + 1] - 4) // 4)
                                        if k0n + 4 < nk and (k0n + 4) not in chunk and (kts[n_ + 1] - 4) % 4 == 0:
                                            load(k0n + 4)
                                        qk(kts[n_ + 1], psi + n_ + 1)
                                    ps = PS[2 + (psi + n_) % 2]; p_ = ptc[n_ % 3]
                                    k0 = 4 + 4 * ((kt - 4) // 4)
                                    v_ = chunk[k0][1]
                                    sc.op("act", lambda e: e.activation(out=p_.t[:, :], in_=ps.t[:, :], func=AF.Exp, scale=0.125,
                                                                        bias=bh.t[:, kt:kt + 1]), reads=[ps, bh], writes=[p_])
                                    sc.op("pe", lambda e: e.matmul(acc.t[:, :], lhsT=v_.t[:, kt - k0, :], rhs=p_.t[:, :],
                                                                   start=(n_ == 0), stop=(n_ == len(kts) - 1)), reads=[v_, p_], writes=[acc])
                                    yield
                                psi += len(kts)
                                attn_epilogue(acc, T1, PS[6], tmps, g_.t[:, :], g_, o_.t[:, :], o_)
                                sc.dma("pool", BR[1024 + 64 * hh:1024 + 64 * (hh + 1), tq], o_.t[:, :], o_, reads=[o_], awrites=[dBR])
                    cg = c_gen()
                    vci_box = [0]

                    def qvars(si, g, ql, qi):
                        Tq = 4 * g + ql
                        nkt = Tq - 3
                        return dict(Tq=Tq, nkt=nkt, N=nkt * 128, nch=(nkt + 3) // 4, iq_=iq[qi % 2], q_=qb[qi % 2], g_=gb[qi % 2], o_=ob[qi % 2],
                                    tcol=slice(si * 512 + ql * 128, si * 512 + (ql + 1) * 128), si=si, ql=ql)

                    def stage1(v):
                        Tq, nkt, N, nch, iq_, q_, g_, o_, tcol, si, ql = (v[k] for k in ("Tq", "nkt", "N", "nch", "iq_", "q_", "g_", "o_", "tcol", "si", "ql"))
                        sc.dma("sp", iq_.t[64:96, :, :], QT[1536:1664, tcol].rearrange("(h c) t -> c h t", c=32), iq_, reads=[dQT], writes=[iq_])
                        sc.dma("sp", q_.t[:, :, :], QT[512:1024, tcol].rearrange("(h d) t -> d h t", d=64), q_, reads=[dQT], writes=[q_])
                        sc.dma("sp", g_.t[:, :, :], GT[512:1024, tcol].rearrange("(h d) t -> d h t", d=64), g_, reads=[dGT], writes=[g_])
                        def sc_mm(c):
                            k0 = 4 + 4 * c
                            w = min(4, Tq + 1 - k0) * 128
                            for hh in range(4):
                                ps = PS[hh % 2]
                                r_ = rr[4 * (c % 2) + hh]
                                sc.op("pe", lambda e: e.matmul(ps.t[:, :w], lhsT=iq_.t[64:96, hh, :], rhs=kb.t[64:96, 0, k0 * 128:k0 * 128 + w],
                                                               start=True, stop=True), reads=[iq_, kb], writes=[ps])
                                sc.op("act", lambda e: e.activation(out=r_.t[:, :w], in_=ps.t[:, :w], func=AF.Relu), reads=[ps], writes=[r_])
                                sc.op("act", lambda e: e.activation(out=r_.t[:, :w], in_=r_.t[:, :w], func=AF.Copy,
                                                                    scale=iwall.t[:, si * 4 + ql, hh:hh + 1]), reads=[r_, iwall], writes=[r_])

                        def acc_mm(c):
                            k0 = 4 + 4 * c
                            w = min(4, Tq + 1 - k0) * 128
                            kc_ = kbc[kbi[0] % 4]; kbi[0] += 1
                            sc.dma("sp", kc_.t[:, :w], kbrow[0:1, k0 * 128:k0 * 128 + w].partition_broadcast(128)[:, 0, :], kc_, writes=[kc_])
                            dst = SCR.t[:, (k0 - 4) * 128:(k0 - 4) * 128 + w]
                            for hh in range(4):
                                r_ = rr[4 * (c % 2) + hh]
                                o_ap = dst if hh == 3 else kc_.t[:, :w]
                                if hh == 3:
                                    sc.op("pool", lambda e: e.tensor_tensor(out=dst, in0=kc_.t[:, :w], in1=r_.t[:, :w], op=ALU.add),
                                          reads=[kc_, r_], awrites=[SCR])
                                else:
                                    sc.op("pool", lambda e: e.tensor_tensor(out=kc_.t[:, :w], in0=kc_.t[:, :w], in1=r_.t[:, :w], op=ALU.add),
                                          reads=[kc_, r_], writes=[kc_])
                        plan = _topk_plan(N)
                        if plan is not None:
                            cs, R = plan
                            nck = (N + cs - 1) // cs
                            ncand = nck * R * 8
                            assert ncand <= NCAND and cs <= 1024, (N, plan)

                        def level1(j):
                            a0 = j * cs; a1 = min(N, a0 + cs)
                            srcap, srcbuf = SCR.t[:, a0:a1], SCR
                            for r in range(R):
                                co = (j * R + r) * 8
                                sc.op("dve", lambda e: e.max(out=cand[0].t[:, co:co + 8], in_=srcap), reads=[srcbuf], awrites=[cand[0]])
                                if r + 1 < R:
                                    nb = wk[r % 2]
                                    sc.op("dve", lambda e: e.match_replace(out=nb.t[:, :a1 - a0], in_to_replace=cand[0].t[:, co:co + 8],
                                                                            in_values=srcap, imm_value=-3.0e38),
                                          reads=[cand[0], srcbuf], writes=[nb])
                                    srcap, srcbuf = nb.t[:, :a1 - a0], nb
                        nextj = 0
                        sc_mm(0)
                        for c in range(nch):
                            if c + 1 < nch:
                                sc_mm(c + 1)
                            acc_mm(c)
                            if c == nch - 1:
                                sc.op("dve", lambda e: e.tensor_tensor(out=SCR.t[:, N - 128:N], in0=SCR.t[:, N - 128:N], in1=CAUS, op=ALU.add),
                                      reads=[SCR, C32], writes=[SCR])
                                cover = N
                            else:
                                cover = min(N - 128, (c + 1) * 512)
                            if plan is not None:
                                while nextj < nck and min(N, (nextj + 1) * cs) <= cover:
                                    level1(nextj); nextj += 1
                        if plan is None:
                            assert N <= 1024
                            cur, curb = SCR.t[:, :N], SCR
                            bufs2 = [(wk[0].t[:, :N], wk[0]), (wk[1].t[:, :N], wk[1])]
                        else:
                            assert nextj == nck
                            cur, curb = cand[0].t[:, :ncand], cand[0]
                            bufs2 = [(cand[1].t[:, :ncand], cand[1]), (cand[0].t[:, :ncand], cand[0])]
                        for r in range(33):
                            sc.op("dve", lambda e: e.max(out=m8.t[:, r * 8:(r + 1) * 8], in_=cur), reads=[curb], writes=[m8])
                            if r < 32:
                                nxt, nxtb = bufs2[r % 2]
                                sc.op("dve", lambda e: e.match_replace(out=nxt, in_to_replace=m8.t[:, r * 8:(r + 1) * 8], in_values=cur,
                                                                        imm_value=-3.0e38), reads=[m8, curb], writes=[nxtb])
                                cur, curb = nxt, nxtb
                        sc.op("dve", lambda e: e.tensor_scalar(out=thr.t[:, 1:2], in0=m8.t[:, 255:256], scalar1=0.5, scalar2=None, op0=ALU.mult),
                              reads=[m8], writes=[thr])
                        sc.op("dve", lambda e: e.scalar_tensor_tensor(out=thr.t[:, 0:1], in0=m8.t[:, 256:257], scalar=0.5, in1=thr.t[:, 1:2],
                                                                      op0=ALU.mult, op1=ALU.add), reads=[m8, thr], writes=[thr])

                    def stage2(v):
                        Tq, nkt, N, nch, iq_, q_, g_, o_, tcol, si, ql = (v[k] for k in ("Tq", "nkt", "N", "nch", "iq_", "q_", "g_", "o_", "tcol", "si", "ql"))
                        sc.op("dve", lambda e: e.tensor_scalar(out=dthr.t[:, :], in0=IDF, scalar1=thr.t[:, 0:1], scalar2=None, op0=ALU.mult),
                              reads=[thr, C32], writes=[dthr])
                        sc.op("pe", lambda e: e.matmul(PS[6].t[:, 0:128], lhsT=ONESF, rhs=dthr.t[:, :], start=True, stop=True),
                              reads=[dthr, C32], writes=[PS[6]])
                        sc.op("dve", lambda e: e.tensor_copy(out=thrbc.t[:, :], in_=PS[6].t[:, 0:128]), reads=[PS[6]], writes=[thrbc])
                        for c in range(nch):
                            k0 = 4 + 4 * c
                            nt_ = min(4, Tq + 1 - k0)
                            ps = PS[c % 2]
                            for t_ in range(nt_):
                                sc.op("pe", lambda e: e.transpose(out=ps.t[:, t_ * 128:(t_ + 1) * 128],
                                                                  in_=SCR.t[:, (k0 - 4 + t_) * 128:(k0 - 3 + t_) * 128], identity=IDF),
                                      reads=[SCR, C32], writes=[ps])
                            sc.op("dve", lambda e: e.tensor_tensor(out=NS.t[:, k0:k0 + nt_, :],
                                                                   in0=ps.t[:, :nt_ * 128].rearrange("p (t q) -> p t q", t=nt_),
                                                                   in1=thrbc.t[:, :].unsqueeze(1).to_broadcast([128, nt_, 128]), op=ALU.is_lt),
                                  reads=[ps, thrbc], awrites=[NS])

                    def stage3(v):
                        Tq, nkt, N, nch, iq_, q_, g_, o_, tcol, si, ql = (v[k] for k in ("Tq", "nkt", "N", "nch", "iq_", "q_", "g_", "o_", "tcol", "si", "ql"))
                        units = [(kt, kv) for kt in range(4, Tq + 1) for kv in range(2)]
                        vcur = {}

                        def qk(u, pidx):
                            kt, kv = u
                            ps = PS[pidx % 2]
                            dd = Tq - kt
                            sc.op("pe", lambda e: e.matmul(ps.t[:, :].rearrange("p (h q) -> p h q", h=4), lhsT=kb.t[:64, kv, kt * 128:(kt + 1) * 128],
                                                           rhs=q_.t[:, 4 * kv:4 * kv + 4, :], start=True, stop=False), reads=[kb, q_], writes=[ps])
                            sc.op("pe", lambda e: e.matmul(ps.t[:, :].rearrange("p (h q) -> p h q", h=4), lhsT=INEG,
                                                           rhs=NS.t[:, kt, :].unsqueeze(1).to_broadcast([128, 4, 128]), start=False, stop=(dd >= 5)),
                                  reads=[CBF, NS], writes=[ps])
                            if dd < 5:
                                sc.op("pe", lambda e: e.matmul(ps.t[:, :].rearrange("p (h q) -> p h q", h=4), lhsT=I8,
                                                               rhs=BMB.t[:, dd, 4 * kv:4 * kv + 4, :], start=False, stop=True),
                                      reads=[CBF, BMB], writes=[ps])
                        def load_chunk(k0):
                            vb_ = vbc[vci_box[0] % 4]; vci_box[0] += 1
                            ntl = min(4, Tq + 1 - k0)
                            sc.dma("sp", vb_.t[:, :ntl, :], VV[k0 * 128:(k0 + ntl) * 128, 128:384].rearrange("(t p) x -> p t x", p=128), vb_,
                                   reads=[dVV], writes=[vb_])
                            return vb_
                        qk(units[0], 0)
                        for n_, (kt, kv) in enumerate(units):
                            if n_ + 1 < len(units):
                                qk(units[n_ + 1], n_ + 1)
                            if (kt - 4) % 4 == 0 and kv == 0:
                                if kt == 4:
                                    vcur[4] = load_chunk(4)
                                if kt + 4 <= Tq:
                                    vcur[kt + 4] = load_chunk(kt + 4)
                                vcur["b"] = vcur[kt]; vcur["k0"] = kt
                            vb_ = vcur["b"]
                            ps = PS[n_ % 2]; p_ = pt[n_ % 4]
                            acc = PS[4 + kv]
                            sc.op("act", lambda e: e.activation(out=p_.t[:, :], in_=ps.t[:, :], func=AF.Exp, scale=0.125,
                                                                bias=KBIAS[:, kt:kt + 1]), reads=[ps, C32], writes=[p_])
                            sc.op("pe", lambda e: e.matmul(acc.t[:, :], lhsT=vb_.t[:, kt - vcur["k0"], kv * 128:(kv + 1) * 128], rhs=p_.t[:, :],
                                                           start=(kt == 4), stop=(kt == Tq)), reads=[vb_, p_], writes=[acc])
                            next(cg, None)
                        for kv in range(2):
                            attn_epilogue(PS[4 + kv], T1, PS[6], tmps, g_.t[:, 4 * kv:4 * kv + 4, :], g_, o_.t[:, 4 * kv:4 * kv + 4, :], o_)
                        sc.dma("pool", BR[512:1024, tcol].rearrange("(h d) t -> d h t", d=64), o_.t[:, :, :], o_, reads=[o_], awrites=[dBR])
                    QL = [qvars(si, g, ql, i4 * 4 + ql) for i4, (si, g) in enumerate(enumerate(own)) for ql in range(4)]
                    stage1(QL[0])
                    for n_q, v in enumerate(QL):
                        stage2(v)
                        if n_q + 1 < len(QL):
                            stage1(QL[n_q + 1])
                        stage3(v)
                    for _ in cg:
                        pass
                    sc.barrier()

                with ExitStack() as st:
                    wb = sb(st, "wb", [128, 12, D], BF16)
                    wo = sb(st, "wo", [128, 8, D], BF16)
                    wst = [sb(st, "wst3_%d" % i, [128, 4, 512]) for i in range(2)]
                    br = [sb(st, "br%d" % i, [128, 12, 512], BF16) for i in range(2)]
                    mg = [sb(st, "mg%d" % i, [128, 3, 512], BF16) for i in range(2)]
                    tm = [sb(st, "tm%d" % i, [128, 512]) for i in range(3)]
                    mgd = [sb(st, "mgd%d" % i, [128, 8, 512], BF16) for i in range(2)]
                    xt = [sb(st, "x3_%d" % i, [128, D]) for i in range(2)]
                    xo = [sb(st, "xo%d" % i, [128, D]) for i in range(2)]
                    ci = 0
                    wbv = w_br[l].rearrange("n (c p) d -> p (n c) d", p=128)
                    wov = w_out[l].rearrange("(c p) d -> p c d", p=128)
                    for (srcv, dstb, nchunk) in [(wbv, wb, 12), (wov, wo, 8)]:
                        for c4 in range(0, nchunk, 4):
                            for d0 in (0, 512):
                                s_ = wst[ci % 2]
                                sc.dma("sp", s_.t[:, :, :], srcv[:, c4:c4 + 4, d0:d0 + 512], s_, writes=[s_])
                                en = ["dve", "pool"][ci % 2]; ci += 1
                                sc.op(en, lambda e: e.tensor_copy(out=dstb.t[:, c4:c4 + 4, d0:d0 + 512], in_=s_.t[:, :, :]), reads=[s_], awrites=[dstb])
                    for si, g in enumerate(own):
                        tq = slice(si * 512, (si + 1) * 512)
                        b_, md = br[si % 2], mgd[si % 2]
                        sc.dma("sp", b_.t[:, :, :], BR[:, tq].rearrange("(c p) t -> p c t", p=128), b_, reads=[dBR], writes=[b_])
                        for dc in range(8):
                            m_ = mg[dc % 2]
                            sc.dma("sp", m_.t[:, :, :], MT[:, tq].rearrange("(n c p) t -> p n c t", p=128, n=3)[:, :, dc, :], m_, reads=[dMT], writes=[m_])
                            for n in range(3):
                                ps = PS[n]
                                for c in range(4):
                                    sc.op("pe", lambda e: e.matmul(ps.t[:, :], lhsT=wb.t[:, 4 * n + c, dc * 128:(dc + 1) * 128], rhs=b_.t[:, 4 * n + c, :],
                                                                   start=(c == 0), stop=(c == 3)), reads=[wb, b_], writes=[ps])
                                sc.op("dve", lambda e: e.tensor_tensor(out=tm[n].t[:, :], in0=ps.t[:, :], in1=m_.t[:, n, :], op=ALU.mult),
                                      reads=[ps, m_], writes=[tm[n]])
                            sc.op("pool", lambda e: e.tensor_tensor(out=tm[0].t[:, :], in0=tm[0].t[:, :], in1=tm[1].t[:, :], op=ALU.add),
                                  reads=[tm[0], tm[1]], writes=[tm[0]])
                            sc.op("pool", lambda e: e.tensor_tensor(out=md.t[:, dc, :], in0=tm[0].t[:, :], in1=tm[2].t[:, :], op=ALU.add),
                                  reads=[tm[0], tm[2]], awrites=[md])
                        for tt in range(4):
                            tile = 4 * g + tt
                            x_, xo_ = xt[tt % 2], xo[tt % 2]
                            sc.dma("sp", x_.t[:, :], xsrc[tile * 128:(tile + 1) * 128, :], x_, reads=([dXS] if dXS else []), writes=[x_])
                            for e2 in range(2):
                                ps = PS[4 + e2]
                                for dc in range(8):
                                    sc.op("pe", lambda e: e.matmul(ps.t[:, :], lhsT=md.t[:, dc, tt * 128:(tt + 1) * 128], rhs=wo.t[:, dc, e2 * 512:(e2 + 1) * 512],
                                                                   start=(dc == 0), stop=(dc == 7)), reads=[md, wo], writes=[ps])
                                sc.op("dve", lambda e: e.tensor_tensor(out=xo_.t[:, e2 * 512:(e2 + 1) * 512], in0=ps.t[:, :], in1=x_.t[:, e2 * 512:(e2 + 1) * 512],
                                                                       op=ALU.add), reads=[ps, x_], awrites=[xo_])
                            if l < depth - 1:
                                sc.dma("pool", X1[tile * 128:(tile + 1) * 128, :], xo_.t[:, :], xo_, reads=[xo_], awrites=[dX1])
                            else:
                                sc.dma("pool", y[(si * 4 + tt) * 128:(si * 4 + tt + 1) * 128, :], xo_.t[:, :], xo_, reads=[xo_], awrites=[dY])
                    sc.barrier()
    return nc, dict(NG=NG, SP=SP, NT=NT, NOUT=NOUT, NC32=NC32, NCBF=NCBF, ninst=sc.ninst, nsem=sc.nsem)


def _t5_bucket_np(delta):
    n = np.maximum(delta, 0)
    nf = np.maximum(n, 1).astype(np.float32)
    large = 16 + (np.log(nf / np.float32(16)) / np.float32(math.log(512 / 16)) * np.float32(16)).astype(np.int32)
    large = np.minimum(large, 31)
    return np.where(n < 16, n, large)


def _host_consts(S, j):
    NGP = S // 512 + 1
    NT = NGP * 4
    pad = 1024 - 512 * j
    k = np.arange(128)[:, None]
    q = np.arange(128)[None, :]
    idf = np.eye(128, dtype=np.float32)
    ones = np.ones((128, 128), np.float32)
    bd = np.zeros((128, 128), np.float32); bd[:64, :64] = 1 / 64; bd[64:, 64:] = 1 / 64
    e0 = np.zeros((128, 128), np.float32); e0[0, :] = 1
    sh = np.zeros((128, 64), np.float32); sh[64 + np.arange(64), np.arange(64)] = 1
    caus = np.where(q > k, np.float32(-1e30), np.float32(0)).astype(np.float32)
    kbias = np.zeros((128, NT), np.float32); kbias[:, :pad // 128] = -30000.0
    kpos = 512 + np.arange(512)
    kpad = np.tile(np.where(kpos < pad, np.float32(-1e30), np.float32(0))[None, :], (128, 1)).astype(np.float32)
    c32 = np.concatenate([idf, ones, bd, e0, sh, caus, kbias, kpad], axis=1).astype(np.float32)
    allpos = np.arange(NGP * 512)
    kbrow = np.where(allpos < pad, np.float32(-1e30), (-(allpos.astype(np.float64)) * 1e-30).astype(np.float32))[None, :].astype(np.float32)
    i8 = 8.0 * idf
    ineg = -30000.0 * idf
    ma0 = np.where(k <= q, 0.0, NEGM)
    ma1 = np.where(k > q, 0.0, NEGM)
    cms = []
    qq = np.arange(512)[None, :]
    for i in range(4):
        cms.append(np.where(128 * i + k <= qq, 0.0, NEGM))
    cbf = np.concatenate([i8, ineg, ma0, ma1] + cms + [idf], axis=1).astype(ml_dtypes.bfloat16)
    return c32, cbf, kbrow


def _t5_tiles(rel_bias_cols, dds):
    k = np.arange(128)[:, None]
    q = np.arange(128)[None, :]
    out = []
    for dd in dds:
        if dd is None:
            b = np.full((128, 128), 31, np.int64)
        else:
            b = _t5_bucket_np(dd * 128 + q - k)
        t = rel_bias_cols[b]
        out.append(np.ascontiguousarray(t.transpose(0, 2, 1)).reshape(128, 1024))
    return np.stack(out).astype(np.float32)


def make_in_maps(S, x, norm_gain, w_in, b_forget, qk_gain, sinks, w_branch, w_out, rel_bias):
    B = x.shape[0]
    depth = w_in.shape[0]
    SP = S + 512
    f = lambda a: np.ascontiguousarray(a, dtype=np.float32)
    gainbc = f(np.broadcast_to(norm_gain[:, None, :], (depth, 128, D)))
    qkg = np.zeros((depth, 128, 8), np.float32)
    for n in range(3):
        for r in range(2):
            qkg[:, :, 2 * n + r] = np.tile(qk_gain[:, n, r, :], (1, 2))
    bfc = np.zeros((depth, 128, 1), np.float32); bfc[:, :8, 0] = b_forget
    sinkrep = f(np.broadcast_to(np.repeat(sinks, 128, axis=1)[:, None, :], (depth, 128, 1024)))
    t5a = _t5_tiles(np.asarray(rel_bias)[:, :8], [0, 1])
    t5b = _t5_tiles(np.asarray(rel_bias)[:, 8:], [0, 1, 2, 3, 4, None])
    consts = [_host_consts(S, j) for j in range(2)]
    maps = []
    for b in range(B):
        for j in range(2):
            pad = 1024 - 512 * j
            ntok = SP - pad
            xs_ = np.zeros((SP, D), np.float32)
            xs_[pad:pad + ntok] = x[b, :ntok]
            maps.append({"xs": xs_, "w_in": f(w_in), "w_br": f(w_branch), "w_out": f(w_out), "gainbc": gainbc, "qkg": qkg,
                         "bfc": bfc, "sinkrep": sinkrep, "t5a": t5a, "t5b": t5b, "c32": consts[j][0], "cbf": consts[j][1], "kbrow": consts[j][2]})
    return maps


def assemble(S, B, results):
    NG = S // 512
    out = np.zeros((B, S, D), np.float32)
    for b in range(B):
        for j in range(2):
            yv = np.asarray(results[2 * b + j]["y"])
            for si in range(NG // 2):
                gg = 2 * si + j
                out[b, gg * 512:(gg + 1) * 512] = yv[si * 512:(si + 1) * 512]
    return out


_CACHE = {}


def kernel(x, norm_gain, w_in, b_forget, qk_gain, sinks, w_branch, w_out, rel_bias):
    x = np.asarray(x, np.float32)
    B, S, _ = x.shape
    args = [np.asarray(a, np.float32) for a in (norm_gain, w_in, b_forget, qk_gain, sinks, w_branch, w_out, rel_bias)]
    if S not in _CACHE:
        _CACHE[S] = build_program(S, depth=args[1].shape[0])
    nc, info = _CACHE[S]
    maps = make_in_maps(S, x, *args)
    res = run_bass_kernel_spmd(nc, maps, core_ids=list(range(len(maps))))
    return assemble(S, B, res.results)
```

```python
import math
import numpy as np
import ml_dtypes
from contextlib import ExitStack
import concourse.bass as bass
import concourse.mybir as mybir
from concourse.bass_utils import run_bass_kernel_spmd

F32 = mybir.dt.float32
BF16 = mybir.dt.bfloat16
AF = mybir.ActivationFunctionType
ALU = mybir.AluOpType

D = 1024
INC = 7852
EPS = 1e-6
NEGM = -3750.0
TOPK = 256


class Buf:
    __slots__ = ("name", "t", "w", "r", "ds")

    def __init__(self, name, t=None):
        self.name, self.t, self.w, self.r, self.ds = name, t, {}, {}, {}


class Sched:
    def __init__(self, nc, es):
        self.nc, self.es = nc, es
        self.eng = {"pe": nc.tensor, "act": nc.scalar, "dve": nc.vector, "pool": nc.gpsimd, "sp": nc.sync}
        self.psem = {k: es.enter_context(nc.semaphore("p_" + k)) for k in ("pe", "act", "dve", "pool")}
        self.pcnt = {k: 0 for k in self.psem}
        self.waited = {k: {} for k in self.eng}
        self.bufs = []
        self.ninst = 0
        self.sempool = {"sp": [], "pool": []}
        self.nsem = 4

    def buf(self, name, t=None):
        b = Buf(name, t)
        self.bufs.append(b)
        return b

    def _wait(self, e, deps):
        for sid, (sem, val) in deps.items():
            if val <= 0 or self.waited[e].get(sid, 0) >= val:
                continue
            if e == "pe" and sem is self.psem["pe"]:
                continue
            self.eng[e].wait_ge(sem, val)
            self.waited[e][sid] = val
            self.ninst += 1

    @staticmethod
    def _merge(deps, d):
        for sid, tok in d.items():
            if sid not in deps or deps[sid][1] < tok[1]:
                deps[sid] = tok

    def _deps(self, reads, writes, awrites):
        deps = {}
        for b in reads:
            self._merge(deps, b.w)
        for b in writes:
            self._merge(deps, b.w)
            self._merge(deps, b.r)
        for b in awrites:
            self._merge(deps, b.r)
        return deps

    def _post(self, sem, val, reads, writes, awrites):
        tok = (sem, val)
        for b in reads:
            b.r[id(sem)] = tok
        for b in writes:
            b.w = {id(sem): tok}
            b.r = {}
        for b in awrites:
            b.w[id(sem)] = tok

    def op(self, e, fn, reads=(), writes=(), awrites=()):
        self._wait(e, self._deps(reads, writes, awrites))
        inst = fn(self.eng[e])
        self.pcnt[e] += 1
        inst.then_inc(self.psem[e], 1)
        self.ninst += 1
        self._post(self.psem[e], self.pcnt[e], reads, writes, awrites)
        return inst

    def dma(self, q, out, in_, owner, reads=(), writes=(), awrites=()):
        if q not in owner.ds:
            if self.sempool[q]:
                owner.ds[q] = list(self.sempool[q].pop())
            else:
                owner.ds[q] = [self.es.enter_context(self.nc.semaphore("d%s_%s" % (q, owner.name))), 0]
                self.nsem += 1
        d = owner.ds[q]
        deps = self._deps(reads, writes, awrites)
        self._merge(deps, {id(d[0]): (d[0], d[1])})
        self._wait(q, deps)
        inst = self.eng[q].dma_start(out=out, in_=in_)
        d[1] += 16
        inst.then_inc(d[0], 16)
        self.ninst += 1
        self._post(d[0], d[1], reads, writes, awrites)
        return inst

    def release(self, b):
        for q, d in b.ds.items():
            self.sempool[q].append((d[0], d[1]))
        b.ds = {}
        if b in self.bufs:
            self.bufs.remove(b)

    def barrier(self):
        toks = {id(s): (s, self.pcnt[k]) for k, s in self.psem.items()}
        for b in self.bufs:
            for d in b.ds.values():
                toks[id(d[0])] = (d[0], d[1])
        for e in self.eng:
            self._wait(e, toks)
        for b in self.bufs:
            b.w, b.r = {}, {}


def _pois_tail(m, k):
    p = math.exp(-m)
    c = p
    for i in range(1, k + 1):
        p *= m / i
        c += p
    return max(0.0, 1.0 - c)


def _topk_plan(n):
    best = (64 * n if n <= 1024 else 10**12, None)
    for cs in (128, 256, 512, 1024):
        nchunk = (n + cs - 1) // cs
        if nchunk < 2:
            continue
        m = TOPK * cs / n
        for r in range(1, 17):
            if 8 * r > cs or 8 * r * nchunk < TOPK + 16:
                continue
            if _pois_tail(m, 8 * r) * nchunk < 1e-10:
                cost = (2 * r - 1) * n + 66 * nchunk * 8 * r + 150 * nchunk * (2 * r - 1)
                if cost < best[0]:
                    best = (cost, (cs, r))
                break
    if False:
        cand = None
        for cs in (128, 256, 512, 1024):
            nchunk = (n + cs - 1) // cs
            m = TOPK * cs / n
            for r in range(1, 17):
                if 8 * r > cs or 8 * r * nchunk < TOPK + 16:
                    continue
                if _pois_tail(m, 8 * r) * nchunk < 1e-10:
                    cost = (2 * r - 1) * n + 66 * nchunk * 8 * r
                    if cand is None or cost < cand[0]:
                        cand = (cost, (cs, r))
                    break
        return cand[1]
    return best[1]


def build_program(S, depth=2, dbg=False):
    NG = S // 512
    NGP = NG + 1
    SP = NGP * 512
    NT = NGP * 4
    own_by_layer = [list(range(1, NG + 1)) if l < depth - 1 else list(range(2, NG + 1, 2)) for l in range(depth)]
    TOKQ = NG * 512
    NOUT = len(own_by_layer[-1])

    nc = bass.Bass("TRN2", target_bir_lowering=False)
    dt_in = lambda n, s, d=F32: nc.dram_tensor(n, list(s), d, kind="ExternalInput").ap()
    dt_sc = lambda n, s, d=BF16: nc.dram_tensor(n, list(s), d, kind="Internal").ap()
    xs = dt_in("xs", [SP, D])
    w_in = dt_in("w_in", [depth, D, INC])
    w_br = dt_in("w_br", [depth, 3, 512, D])
    w_out = dt_in("w_out", [depth, D, D])
    gainbc = dt_in("gainbc", [depth, 128, D])
    qkg = dt_in("qkg", [depth, 128, 8])
    bfc = dt_in("bfc", [depth, 128, 1])
    sinkrep = dt_in("sinkrep", [depth, 128, 1024])
    t5a = dt_in("t5a", [2, 128, 1024])
    t5b = dt_in("t5b", [6, 128, 1024])
    NC32 = 128 * 4 + 64 + 128 + NT + 512
    c32 = dt_in("c32", [128, NC32])
    kbrow = dt_in("kbrow", [1, SP])
    NCBF = 128 * 5 + 4 * 512
    cbf = dt_in("cbf", [128, NCBF], BF16)
    y = nc.dram_tensor("y", [NOUT * 512, D], F32, kind="ExternalOutput").ap()

    QT = dt_sc("QT", [1664, TOKQ])
    KT = dt_sc("KT", [800, SP])
    VV = dt_sc("VV", [SP, 896])
    GT = dt_sc("GT", [1536, TOKQ])
    MT = dt_sc("MT", [3072, TOKQ])
    BR = dt_sc("BR", [1536, TOKQ])
    X1 = dt_sc("X1", [SP, D], F32)
    dbgo = {}

    es = ExitStack()
    with es:
        sc = Sched(nc, es)

        uid = [0]

        def sb(st, name, shape, dt=F32):
            uid[0] += 1
            name = "%s_%d" % (name, uid[0])
            b = sc.buf(name, st.enter_context(nc.sbuf_tensor(name, list(shape), dt)))
            if st is not es:
                st.callback(sc.release, b)
            return b

        def pst(st, name, shape, dt=F32):
            return sc.buf(name, st.enter_context(nc.psum_tensor(name, list(shape), dt)))

        dQT, dKT, dVV, dGT, dMT, dBR, dX1, dY = (sc.buf(n) for n in ("dQT", "dKT", "dVV", "dGT", "dMT", "dBR", "dX1", "dY"))

        C32 = sb(es, "C32", [128, NC32])
        CBF = sb(es, "CBF", [128, NCBF], BF16)
        o = 0
        IDF = C32.t[:, 0:128]; ONESF = C32.t[:, 128:256]; BDF = C32.t[:, 256:384]; E0F = C32.t[:, 384:512]
        SHF = C32.t[:, 512:576]; CAUS = C32.t[:, 576:704]
        KBIAS = C32.t[:, 704:704 + NT]; KPAD = C32.t[:, 704 + NT:704 + NT + 512]
        I8 = CBF.t[:, 0:128]; INEG = CBF.t[:, 128:256]; MA0 = CBF.t[:, 256:384]; MA1 = CBF.t[:, 384:512]
        CM = [CBF.t[:, 512 + 512 * i:512 + 512 * (i + 1)] for i in range(4)]
        IDB = CBF.t[:, 2560:2688]
        ONE8 = sb(es, "ONE8", [8, 512])
        EPSC = sb(es, "EPSC", [128, 1])
        PS = [pst(es, "ps%d" % i, [128, 512]) for i in range(7)]
        PSB = pst(es, "psb", [128, 1024], BF16)

        sc.dma("sp", C32.t[:, :], c32[:, :], C32, writes=[C32])
        sc.dma("sp", CBF.t[:, :], cbf[:, :], CBF, writes=[CBF])
        sc.op("pool", lambda e: e.memset(ONE8.t[:, :], 1.0), writes=[ONE8])
        sc.op("pool", lambda e: e.memset(EPSC.t[:, :], 1e-18), writes=[EPSC])
        sc.barrier()

        def build_bma(st):
            BMA = sb(st, "BMA", [128, 2, 8, 128], BF16)
            tmp = sb(st, "t5tmp", [128, 1024])
            for dd in range(2):
                sc.dma("sp", tmp.t[:, :], t5a[dd], tmp, writes=[tmp])
                msk = MA0 if dd == 0 else MA1
                sc.op("dve", lambda e: e.tensor_tensor(
                    out=BMA.t[:, dd], in0=tmp.t[:, :].rearrange("p (h q) -> p h q", h=8),
                    in1=msk.unsqueeze(1).to_broadcast([128, 8, 128]), op=ALU.add), reads=[tmp, CBF], awrites=[BMA])
            return BMA

        def build_bmb(st):
            BMB = sb(st, "BMB", [128, 5, 8, 128], BF16)
            with ExitStack() as st2:
                tmp = sb(st2, "t5tmpb", [128, 1024])
                tfar = sb(st2, "t5far", [128, 1024])
                sc.dma("sp", tfar.t[:, :], t5b[5], tfar, writes=[tfar])
                for dd in range(5):
                    sc.dma("sp", tmp.t[:, :], t5b[dd], tmp, writes=[tmp])
                    sc.op("dve", lambda e: e.tensor_tensor(out=tmp.t[:, :], in0=tmp.t[:, :], in1=tfar.t[:, :], op=ALU.subtract),
                          reads=[tfar, tmp], writes=[tmp])
                    if dd == 0:
                        sc.op("dve", lambda e: e.tensor_tensor(
                            out=BMB.t[:, 0], in0=tmp.t[:, :].rearrange("p (h q) -> p h q", h=8),
                            in1=MA0.unsqueeze(1).to_broadcast([128, 8, 128]), op=ALU.add), reads=[tmp, CBF], awrites=[BMB])
                    else:
                        sc.op("dve", lambda e: e.tensor_copy(
                            out=BMB.t[:, dd], in_=tmp.t[:, :].rearrange("p (h q) -> p h q", h=8)), reads=[tmp], awrites=[BMB])
                sc.barrier()
            return BMB

        def attn_epilogue(acc, T1, dps, tmps, gate_ap, gate_buf, out_ap, out_buf, sink_ap=None, sink_buf=None):
            d2, ln, rd, t2 = tmps
            sc.op("act", lambda e: e.activation(out=T1.t[:, :], in_=acc.t[:, :], func=AF.Copy), reads=[acc], writes=[T1])
            sc.op("pe", lambda e: e.matmul(dps.t[:64, :], lhsT=SHF, rhs=T1.t[:, :], start=True, stop=True),
                  reads=[T1, C32], writes=[dps])
            if sink_ap is not None:
                sc.op("dve", lambda e: e.tensor_tensor(out=d2.t[:64, :], in0=dps.t[:64, :], in1=sink_ap, op=ALU.add),
                      reads=[dps, sink_buf], writes=[d2])
                sc.op("act", lambda e: e.activation(out=ln.t[:64, :], in_=d2.t[:64, :], func=AF.Ln), reads=[d2], writes=[ln])
            else:
                sc.op("act", lambda e: e.activation(out=ln.t[:64, :], in_=dps.t[:64, :], func=AF.Ln, bias=EPSC.t[:64, 0:1]),
                      reads=[dps, EPSC], writes=[ln])
            sc.op("act", lambda e: e.activation(out=rd.t[:64, :], in_=ln.t[:64, :], func=AF.Exp, scale=-1.0),
                  reads=[ln], writes=[rd])
            sc.op("pool", lambda e: e.tensor_tensor(out=t2.t[:64, :], in0=T1.t[:64, :], in1=rd.t[:64, :], op=ALU.mult),
                  reads=[T1, rd], writes=[t2])
            sc.op("pool", lambda e: e.tensor_tensor(out=out_ap, in0=t2.t[:64, :], in1=gate_ap, op=ALU.mult),
                  reads=[t2, gate_buf], awrites=[out_buf])

        for l in range(depth):
            own = own_by_layer[l]
            nown = len(own)
            xsrc, dXS = (xs, None) if l == 0 else (X1, dX1)
            xdst, dXD = (X1, dX1) if l < depth - 1 else (y, dY)
            lst = ExitStack()
            with lst:
                ckey = sb(lst, "ckey", [128, NT, 8])
                crefbc = sb(lst, "crefbc", [128, NGP, 8])
                iwall = sb(lst, "iwall", [128, nown * 4, 4])
                qkgs = sb(lst, "qkgs", [128, 8])
                sc.dma("sp", qkgs.t[:, :], qkg[l], qkgs, writes=[qkgs])

                for (cbase, ccount, pname) in [(0, 4780, "main"), (4780, 3072, "merge")]:
                  with ExitStack() as st:
                    Wbf = sb(st, "Wbf", [128, 8, ccount], BF16)
                    wst = [sb(st, "wst%d" % i, [128, 8, 128]) for i in range(2)]
                    gbc = sb(st, "gbc", [128, D])
                    bfs = sb(st, "bfs", [128, 1])
                    xts = [sb(st, "xt%d" % i, [128, D]) for i in range(2)]
                    junk = sb(st, "junk", [128, D], BF16)
                    hb = [sb(st, "hb%d" % i, [128, D], BF16) for i in range(2)]
                    hT = [sb(st, "hT%d" % i, [128, 8, 512], BF16) for i in range(2)]
                    ss = [sb(st, "ss%d" % i, [128, 4]) for i in range(2)]
                    sq = sb(st, "sq", [128, 512]); lnb = sb(st, "lnb", [128, 512]); rsb = sb(st, "rsb", [128, 512])
                    stg = [sb(st, "stg%d" % i, [128, 512], BF16) for i in range(4)]
                    vst = [sb(st, "vst%d" % i, [128, 1024], BF16) for i in range(2)]
                    fz = sb(st, "fz", [8, 512]); fe = sb(st, "fe", [8, 512]); fsp = sb(st, "fsp", [8, 512])
                    cg = sb(st, "cg", [8, 512]); cprev = sb(st, "cprev", [8, 1])
                    sc.dma("sp", gbc.t[:, :], gainbc[l], gbc, writes=[gbc])
                    sc.dma("sp", bfs.t[:, :], bfc[l], bfs, writes=[bfs])
                    sc.op("dve", lambda e: e.memset(cprev.t[:, :], 0.0), writes=[cprev])
                    for v_ in vst:
                        sc.op("pool", lambda e: e.memset(v_.t[:, :], 1.0), writes=[v_])
                    wv = w_in[l].rearrange("(c p) n -> p c n", p=128)
                    ceng = ["dve", "pool", "act"]
                    for ci, c0 in enumerate(range(0, ccount, 128)):
                        cw = min(128, ccount - c0)
                        s_ = wst[ci % 2]
                        sc.dma("sp", s_.t[:, :, :cw], wv[:, :, cbase + c0:cbase + c0 + cw], s_, writes=[s_])
                        en = ceng[ci % 3]
                        if en == "act":
                            sc.op("act", lambda e: e.activation(out=Wbf.t[:, :, c0:c0 + cw], in_=s_.t[:, :, :cw], func=AF.Copy),
                                  reads=[s_], awrites=[Wbf])
                        else:
                            sc.op(en, lambda e: e.tensor_copy(out=Wbf.t[:, :, c0:c0 + cw], in_=s_.t[:, :, :cw]),
                                  reads=[s_], awrites=[Wbf])
                    stg_i = [0]
                    psr = [0]

                    def nextps():
                        psr[0] = (psr[0] + 1) % 4
                        return PS[psr[0]]

                    def store_fm(src_ap_fn, M, dram_ap, dbuf):
                        s_ = stg[stg_i[0] % 4]; stg_i[0] += 1
                        src_ap_fn(s_)
                        sc.dma("pool", dram_ap, s_.t[:M, :], s_, reads=[s_], awrites=[dbuf])

                    deferred = []

                    def flush_deferred():
                        while deferred:
                            deferred.pop(0)()

                    def fm_mm(ps, c0, M, h):
                        c0 = c0 - cbase
                        for c in range(8):
                            sc.op("pe", lambda e: e.matmul(ps.t[:M, :], lhsT=Wbf.t[:, c, c0:c0 + M], rhs=h.t[:, c, :],
                                                           start=(c == 0), stop=(c == 7)), reads=[Wbf, h], writes=[ps])
                        flush_deferred()

                    def ep_norm(ps, gcol, dram_ap, dbuf):
                        flush_deferred()
                        sc.op("act", lambda e: e.activation(out=sq.t[:, :], in_=ps.t[:, :], func=AF.Square), reads=[ps], writes=[sq])
                        deferred.append(lambda: ep_norm_rest(ps, gcol, dram_ap, dbuf))

                    def ep_norm_rest(ps, gcol, dram_ap, dbuf):
                        sc.op("pe", lambda e: e.matmul(PS[4].t[:, :], lhsT=BDF, rhs=sq.t[:, :], start=True, stop=True),
                              reads=[sq, C32], writes=[PS[4]])
                        sc.op("dve", lambda e: e.tensor_scalar(out=lnb.t[:, :], in0=PS[4].t[:, :], scalar1=EPS, scalar2=None, op0=ALU.add),
                              reads=[PS[4]], writes=[lnb])
                        sc.op("act", lambda e: e.activation(out=rsb.t[:, :], in_=lnb.t[:, :], func=AF.Ln), reads=[lnb], writes=[rsb])
                        sc.op("act", lambda e: e.activation(out=lnb.t[:, :], in_=rsb.t[:, :], func=AF.Exp, scale=-0.5), reads=[rsb], writes=[lnb])
                        store_fm(lambda s_: sc.op("dve", lambda e: e.scalar_tensor_tensor(
                            out=s_.t[:, :], in0=ps.t[:, :], scalar=qkgs.t[:, gcol:gcol + 1], in1=lnb.t[:, :], op0=ALU.mult, op1=ALU.mult),
                            reads=[ps, qkgs, lnb], writes=[s_]), 128, dram_ap, dbuf)

                    def ep_act(ps, M, func, dram_ap, dbuf):
                        store_fm(lambda s_: sc.op("act", lambda e: e.activation(out=s_.t[:M, :], in_=ps.t[:M, :], func=func),
                                                  reads=[ps], writes=[s_]), M, dram_ap, dbuf)

                    for g in range(1, NG + 1):
                        h = hT[g % 2]
                        isown = g in own
                        si = own.index(g) if isown else -1
                        if pname == "merge" and not isown:
                            continue
                        for tt in range(4):
                            tile = 4 * g + tt
                            xt = xts[tt % 2]; hbb = hb[tt % 2]; s4 = ss[tt % 2]
                            sc.dma("sp", xt.t[:, :], xsrc[tile * 128:(tile + 1) * 128, :], xt, reads=([dXS] if dXS else []), writes=[xt])
                            sc.op("act", lambda e: e.activation(out=junk.t[:, :], in_=xt.t[:, :], func=AF.Square, accum_out=s4.t[:, 0:1]),
                                  reads=[xt], writes=[junk, s4])
                            sc.op("dve", lambda e: e.tensor_scalar(out=s4.t[:, 1:2], in0=s4.t[:, 0:1], scalar1=1.0 / D, scalar2=EPS,
                                                                    op0=ALU.mult, op1=ALU.add), reads=[s4], writes=[s4])
                            sc.op("act", lambda e: e.activation(out=s4.t[:, 2:3], in_=s4.t[:, 1:2], func=AF.Ln), reads=[s4], writes=[s4])
                            sc.op("act", lambda e: e.activation(out=s4.t[:, 3:4], in_=s4.t[:, 2:3], func=AF.Exp, scale=-0.5), reads=[s4], writes=[s4])
                            sc.op("dve", lambda e: e.scalar_tensor_tensor(out=hbb.t[:, :], in0=xt.t[:, :], scalar=s4.t[:, 3:4], in1=gbc.t[:, :],
                                                                           op0=ALU.mult, op1=ALU.mult), reads=[xt, s4, gbc], writes=[hbb])
                            for c in range(8):
                                sc.op("pe", lambda e: e.transpose(out=PSB.t[:, c * 128:(c + 1) * 128], in_=hbb.t[:, c * 128:(c + 1) * 128],
                                                                  identity=IDB), reads=[hbb, CBF], writes=[PSB])
                            sc.op("dve", lambda e: e.tensor_copy(out=h.t[:, :, tt * 128:(tt + 1) * 128],
                                                                 in_=PSB.t[:, :].rearrange("p (c t) -> p c t", c=8)), reads=[PSB], awrites=[h])
                        tk = slice(g * 512, (g + 1) * 512)
                        if pname == "merge":
                            tq = slice(si * 512, (si + 1) * 512)
                            for i in range(24):
                                ps = nextps(); fm_mm(ps, 4780 + 128 * i, 128, h); ep_act(ps, 128, AF.Sigmoid, MT[128 * i:128 * (i + 1), tq], dMT)
                            continue
                        for (c0, gcol, r0) in [(512, 1, 0), (1792, 3, 128)] + [(3236 + 128 * i, 5, 256 + 128 * i) for i in range(4)]:
                            ps = nextps(); fm_mm(ps, c0, 128, h); ep_norm(ps, gcol, KT[r0:r0 + 128, tk], dKT)
                        ps = nextps(); fm_mm(ps, 2176, 32, h); ep_act(ps, 32, AF.Copy, KT[768:800, tk], dKT)
                        ps = nextps(); fm_mm(ps, 4260, 8, h)
                        sc.op("dve", lambda e: e.tensor_scalar(out=fz.t[:, :], in0=ps.t[:8, :], scalar1=bfs.t[:8, 0:1], scalar2=None, op0=ALU.add),
                              reads=[ps, bfs], writes=[fz])
                        sc.op("act", lambda e: e.activation(out=fe.t[:, :], in_=fz.t[:, :], func=AF.Exp, scale=-1.0), reads=[fz], writes=[fe])
                        sc.op("dve", lambda e: e.tensor_scalar(out=fz.t[:, :], in0=fe.t[:, :], scalar1=1.0, scalar2=None, op0=ALU.add),
                              reads=[fe], writes=[fz])
                        sc.op("act", lambda e: e.activation(out=fsp.t[:, :], in_=fz.t[:, :], func=AF.Ln), reads=[fz], writes=[fsp])
                        sc.op("dve", lambda e: e.tensor_tensor_scan(out=cg.t[:, :], data0=ONE8.t[:, :], data1=fsp.t[:, :], initial=cprev.t[:, 0:1],
                                                                     op0=ALU.mult, op1=ALU.subtract), reads=[ONE8, fsp, cprev], writes=[cg])
                        sc.op("dve", lambda e: e.tensor_copy(out=cprev.t[:, :], in_=cg.t[:, 511:512]), reads=[cg], writes=[cprev])
                        for tt in range(4):
                            sc.op("pe", lambda e: e.transpose(out=PS[5].t[:, tt * 8:(tt + 1) * 8], in_=cg.t[:8, tt * 128:(tt + 1) * 128],
                                                              identity=C32.t[:8, 0:8]), reads=[cg, C32], writes=[PS[5]])
                        sc.op("dve", lambda e: e.tensor_copy(out=ckey.t[:, 4 * g:4 * g + 4, :], in_=PS[5].t[:, 0:32].rearrange("p (t h) -> p t h", t=4)),
                              reads=[PS[5]], awrites=[ckey])
                        sc.op("pe", lambda e: e.matmul(PS[5].t[:, 32:40], lhsT=E0F, rhs=ckey.t[:, 4 * g, :], start=True, stop=True),
                              reads=[ckey, C32], writes=[PS[5]])
                        sc.op("dve", lambda e: e.tensor_copy(out=crefbc.t[:, g, :], in_=PS[5].t[:, 32:40]), reads=[PS[5]], awrites=[crefbc])
                        flush_deferred()
                        for tt in range(4):
                            tile = 4 * g + tt
                            vs_ = vst[tt % 2]
                            for (c0, n, pb, o0) in [(640, 128, PS[5], 0), (1920, 128, PS[5], 128), (3748, 512, PS[6], 0)]:
                                for c in range(8):
                                    sc.op("pe", lambda e: e.matmul(pb.t[:, o0:o0 + n], lhsT=h.t[:, c, tt * 128:(tt + 1) * 128],
                                                                   rhs=Wbf.t[:, c, c0 - cbase:c0 - cbase + n], start=(c == 0), stop=(c == 7)),
                                          reads=[Wbf, h], writes=[pb])
                            sc.op("dve", lambda e: e.tensor_copy(out=vs_.t[:, 0:128], in_=PS[5].t[:, 0:128]), reads=[PS[5]], awrites=[vs_])
                            sc.op("dve", lambda e: e.tensor_copy(out=vs_.t[:, 128:384].rearrange("p (h x) -> p h x", h=2)[:, :, 0:64],
                                                                 in_=PS[5].t[:, 128:256].rearrange("p (h d) -> p h d", h=2)), reads=[PS[5]], awrites=[vs_])
                            sc.op("act", lambda e: e.activation(out=vs_.t[:, 384:896], in_=PS[6].t[:, :], func=AF.Copy), reads=[PS[6]], awrites=[vs_])
                            sc.dma("pool", VV[tile * 128:(tile + 1) * 128, :], vs_.t[:, 0:896], vs_, reads=[vs_], awrites=[dVV])
                            if isown:
                                for c in range(8):
                                    sc.op("pe", lambda e: e.matmul(PS[5].t[:, 256:260], lhsT=h.t[:, c, tt * 128:(tt + 1) * 128],
                                                                   rhs=Wbf.t[:, c, 2208 - cbase:2212 - cbase], start=(c == 0), stop=(c == 7)),
                                          reads=[Wbf, h], writes=[PS[5]])
                                sc.op("dve", lambda e: e.tensor_copy(out=iwall.t[:, si * 4 + tt, :], in_=PS[5].t[:, 256:260]),
                                      reads=[PS[5]], awrites=[iwall])
                        if not isown:
                            continue
                        tq = slice(si * 512, (si + 1) * 512)
                        for (b0, gcol, r0) in [(0, 0, 0), (1280, 2, 512), (2724, 4, 1024)]:
                            for i in range(4):
                                ps = nextps(); fm_mm(ps, b0 + 128 * i, 128, h); ep_norm(ps, gcol, QT[r0 + 128 * i:r0 + 128 * (i + 1), tq], dQT)
                        ps = nextps(); fm_mm(ps, 2048, 128, h); ep_act(ps, 128, AF.Copy, QT[1536:1664, tq], dQT)
                        for (b0, r0) in [(768, 0), (2212, 512), (4268, 1024)]:
                            for i in range(4):
                                ps = nextps(); fm_mm(ps, b0 + 128 * i, 128, h); ep_act(ps, 128, AF.Silu, GT[r0 + 128 * i:r0 + 128 * (i + 1), tq], dGT)
                        flush_deferred()
                    flush_deferred()
                    sc.barrier()

                with ExitStack() as st:
                    BMA = build_bma(st)
                    sinkx = sb(st, "sinkx", [128, 1024])
                    sc.dma("sp", sinkx.t[:, :], sinkrep[l], sinkx, writes=[sinkx])
                    sc.op("act", lambda e: e.activation(out=sinkx.t[:, :], in_=sinkx.t[:, :], func=AF.Exp), reads=[sinkx], writes=[sinkx])
                    qa = [sb(st, "qa%d" % i, [64, 8, 512], BF16) for i in range(2)]
                    ga = [sb(st, "ga%d" % i, [64, 8, 512], BF16) for i in range(2)]
                    ka = [sb(st, "ka%d" % i, [64, 2, 640], BF16) for i in range(2)]
                    va = [sb(st, "va%d" % i, [128, 5, 2, 128], BF16) for i in range(2)]
                    oa = [sb(st, "oa%d" % i, [64, 8, 512], BF16) for i in range(2)]
                    pt = [sb(st, "pt%d" % i, [128, 512], BF16) for i in range(3)]
                    T1 = sb(st, "T1", [128, 512]); tmps = [sb(st, "ta%d" % i, [64, 512]) for i in range(4)]
                    for v_ in va:
                        sc.op("pool", lambda e, v_=v_: e.memset(v_.t[:, :, :, 64:128], 1.0), awrites=[v_])
                    pti = 0
                    for si, g in enumerate(own):
                        q_, g_, k_, v_, o_ = qa[si % 2], ga[si % 2], ka[si % 2], va[si % 2], oa[si % 2]
                        tq = slice(si * 512, (si + 1) * 512)
                        sc.dma("sp", q_.t[:, :, :], QT[0:512, tq].rearrange("(h d) t -> d h t", d=64), q_, reads=[dQT], writes=[q_])
                        sc.dma("sp", g_.t[:, :, :], GT[0:512, tq].rearrange("(h d) t -> d h t", d=64), g_, reads=[dGT], writes=[g_])
                        t0 = 4 * g - 1
                        f0 = 1 if g == 1 else 0
                        sc.dma("sp", k_.t[:, :, f0 * 128:], KT[0:128, (t0 + f0) * 128:(t0 + 5) * 128].rearrange("(h d) t -> d h t", d=64), k_,
                               reads=[dKT], writes=[k_])
                        for kv in range(2):
                            sc.dma("sp", v_.t[:, f0:, kv, 0:64], VV[(t0 + f0) * 128:(t0 + 5) * 128, 64 * kv:64 * (kv + 1)].rearrange("(t p) d -> p t d", p=128), v_,
                                   reads=[dVV], awrites=[v_])
                        for kv in range(2):
                            for ql in range(4):
                                acc = PS[4 + (ql % 2)]
                                kts = [ql, ql + 1] if not (g == 1 and ql == 0) else [ql + 1]
                                for n_, ktl in enumerate(kts):
                                    ps = PS[pti % 3]; p_ = pt[pti % 3]; pti += 1
                                    dd = 1 if ktl == ql else 0
                                    sc.op("pe", lambda e: e.matmul(ps.t[:, :].rearrange("p (h q) -> p h q", h=4), lhsT=k_.t[:, kv, ktl * 128:(ktl + 1) * 128],
                                                                   rhs=q_.t[:, 4 * kv:4 * kv + 4, ql * 128:(ql + 1) * 128], start=True, stop=False),
                                          reads=[k_, q_], writes=[ps])
                                    sc.op("pe", lambda e: e.matmul(ps.t[:, :].rearrange("p (h q) -> p h q", h=4), lhsT=I8,
                                                                   rhs=BMA.t[:, dd, 4 * kv:4 * kv + 4, :], start=False, stop=True),
                                          reads=[CBF, BMA], writes=[ps])
                                    kt_abs = t0 + ktl
                                    sc.op("act", lambda e: e.activation(out=p_.t[:, :], in_=ps.t[:, :], func=AF.Exp, scale=0.125,
                                                                        bias=KBIAS[:, kt_abs:kt_abs + 1]), reads=[ps, C32], writes=[p_])
                                    sc.op("pe", lambda e: e.matmul(acc.t[:, :], lhsT=v_.t[:, ktl, kv, :], rhs=p_.t[:, :],
                                                                   start=(n_ == 0), stop=(n_ == len(kts) - 1)), reads=[v_, p_], writes=[acc])
                                attn_epilogue(acc, T1, PS[6], tmps,
                                              g_.t[:, 4 * kv:4 * kv + 4, ql * 128:(ql + 1) * 128],
                                              g_, o_.t[:, 4 * kv:4 * kv + 4, ql * 128:(ql + 1) * 128], o_,
                                              sink_ap=sinkx.t[:64, kv * 512:(kv + 1) * 512], sink_buf=sinkx)
                        sc.dma("pool", BR[0:512, tq].rearrange("(h d) t -> d h t", d=64), o_.t[:, :, :], o_, reads=[o_], awrites=[dBR])
                    sc.barrier()

                with ExitStack() as st:
                    kc = [sb(st, "kc%d" % i, [64, SP], BF16) for i in range(2)]
                    vc = [sb(st, "vc%d" % i, [128, NT, 128], BF16) for i in range(2)]
                    qc = [sb(st, "qc%d" % i, [64, 512], BF16) for i in range(2)]
                    gc = [sb(st, "gc%d" % i, [64, 512], BF16) for i in range(2)]
                    oc = [sb(st, "oc%d" % i, [64, 512], BF16) for i in range(2)]
                    bias_h = [sb(st, "biash%d" % i, [128, NT]) for i in range(2)]
                    pt = [sb(st, "ptc%d" % i, [128, 512], BF16) for i in range(3)]
                    T1 = sb(st, "T1c", [128, 512]); tmps = [sb(st, "tc%d" % i, [64, 512]) for i in range(4)]
                    for v_ in vc:
                        sc.op("pool", lambda e, v_=v_: e.memset(v_.t[:, :, 64:128], 1.0), awrites=[v_])
                    it = 0
                    for hh in range(8):
                        k_, v_ = kc[hh % 2], vc[hh % 2]
                        sc.dma("sp", k_.t[:, 512:], KT[256 + 64 * hh:256 + 64 * (hh + 1), 512:], k_, reads=[dKT], writes=[k_])
                        sc.dma("sp", v_.t[:, 4:, 0:64], VV[512:, 384 + 64 * hh:384 + 64 * (hh + 1)].rearrange("(t p) d -> p t d", p=128), v_,
                               reads=[dVV], awrites=[v_])
                        for si, g in enumerate(own):
                            q_, g_, o_, bh = qc[it % 2], gc[it % 2], oc[it % 2], bias_h[it % 2]
                            acc = PS[4 + (it % 2)]
                            it += 1
                            tq = slice(si * 512, (si + 1) * 512)
                            sc.dma("sp", q_.t[:, :], QT[1024 + 64 * hh:1024 + 64 * (hh + 1), tq], q_, reads=[dQT], writes=[q_])
                            sc.dma("sp", g_.t[:, :], GT[1024 + 64 * hh:1024 + 64 * (hh + 1), tq], g_, reads=[dGT], writes=[g_])
                            nk = 4 * g + 4
                            sc.op("dve", lambda e: e.tensor_scalar(out=bh.t[:, 4:nk], in0=ckey.t[:, 4:nk, hh], scalar1=crefbc.t[:, g, hh:hh + 1],
                                                                    scalar2=-1.0, op0=ALU.subtract, op1=ALU.mult), reads=[ckey, crefbc], writes=[bh])
                            sc.op("dve", lambda e: e.tensor_tensor(out=bh.t[:, 4:nk], in0=bh.t[:, 4:nk], in1=KBIAS[:, 4:nk], op=ALU.add),
                                  reads=[bh, C32], writes=[bh])
                            kts = list(range(4, nk))

                            def qk(kt, pidx):
                                ps = PS[pidx % 3]
                                diag = kt >= 4 * g
                                sc.op("pe", lambda e: e.matmul(ps.t[:, :], lhsT=k_.t[:, kt * 128:(kt + 1) * 128], rhs=q_.t[:, :],
                                                               start=True, stop=not diag), reads=[k_, q_], writes=[ps])
                                if diag:
                                    sc.op("pe", lambda e: e.matmul(ps.t[:, :], lhsT=I8, rhs=CM[kt - 4 * g], start=False, stop=True),
                                          reads=[CBF], writes=[ps])
                            qk(kts[0], 0)
                            for n_, kt in enumerate(kts):
                                if n_ + 1 < len(kts):
                                    qk(kts[n_ + 1], n_ + 1)
                                ps = PS[n_ % 3]; p_ = pt[n_ % 3]
                                sc.op("act", lambda e: e.activation(out=p_.t[:, :], in_=ps.t[:, :], func=AF.Exp, scale=0.125,
                                                                    bias=bh.t[:, kt:kt + 1]), reads=[ps, bh], writes=[p_])
                                sc.op("pe", lambda e: e.matmul(acc.t[:, :], lhsT=v_.t[:, kt, :], rhs=p_.t[:, :],
                                                               start=(n_ == 0), stop=(n_ == len(kts) - 1)), reads=[v_, p_], writes=[acc])
                            attn_epilogue(acc, T1, PS[6], tmps, g_.t[:, :], g_, o_.t[:, :], o_)
                            sc.dma("pool", BR[1024 + 64 * hh:1024 + 64 * (hh + 1), tq], o_.t[:, :], o_, reads=[o_], awrites=[dBR])
                    sc.barrier()

                with ExitStack() as st:
                    BMB = build_bmb(st)
                    NKEY = (NT - 4) * 128
                    SCR = sb(st, "SCR", [128, NKEY])
                    NS = sb(st, "NS", [128, NT, 128], BF16)
                    kb = sb(st, "kb", [128, 2, SP], BF16)
                    vbc = [sb(st, "vbc%d" % i, [128, 4, 256], BF16) for i in range(4)]
                    iq = [sb(st, "iq%d" % i, [128, 4, 128], BF16) for i in range(2)]
                    qb = [sb(st, "qb%d" % i, [64, 8, 128], BF16) for i in range(2)]
                    gb = [sb(st, "gb%d" % i, [64, 8, 128], BF16) for i in range(2)]
                    ob = [sb(st, "ob%d" % i, [64, 8, 128], BF16) for i in range(2)]
                    rr = [sb(st, "rr%d" % i, [128, 512]) for i in range(8)]
                    kbc = [sb(st, "kbc%d" % i, [128, 512]) for i in range(4)]
                    kbi = [0]
                    dw = sb(st, "dw", [128, 4, 128])
                    pt = [sb(st, "ptb%d" % i, [128, 512], BF16) for i in range(4)]
                    T1 = sb(st, "T1b", [128, 512]); tmps = [sb(st, "tb%d" % i, [64, 512]) for i in range(4)]
                    NCAND = 1152
                    cand = [sb(st, "cand%d" % i, [128, NCAND]) for i in range(2)]
                    wk = [sb(st, "wk%d" % i, [128, 1024]) for i in range(2)]
                    m8 = sb(st, "m8", [128, 8 * 34])
                    thr = sb(st, "thr", [128, 2]); dthr = sb(st, "dthr", [128, 128]); thrbc = sb(st, "thrbc", [128, 128])
                    sc.dma("sp", kb.t[:64, :, 512:], KT[128:256, 512:].rearrange("(h d) t -> d h t", d=64), kb, reads=[dKT], awrites=[kb])
                    sc.dma("sp", kb.t[64:96, 0, 512:], KT[768:800, 512:], kb, reads=[dKT], awrites=[kb])
                    sc.barrier()
                    vci_box = [0]

                    def qvars(si, g, ql, qi):
                        Tq = 4 * g + ql
                        nkt = Tq - 3
                        return dict(Tq=Tq, nkt=nkt, N=nkt * 128, nch=(nkt + 3) // 4, iq_=iq[qi % 2], q_=qb[qi % 2], g_=gb[qi % 2], o_=ob[qi % 2],
                                    tcol=slice(si * 512 + ql * 128, si * 512 + (ql + 1) * 128), si=si, ql=ql)

                    def stage1(v):
                        Tq, nkt, N, nch, iq_, q_, g_, o_, tcol, si, ql = (v[k] for k in ("Tq", "nkt", "N", "nch", "iq_", "q_", "g_", "o_", "tcol", "si", "ql"))
                        sc.dma("sp", iq_.t[64:96, :, :], QT[1536:1664, tcol].rearrange("(h c) t -> c h t", c=32), iq_, reads=[dQT], writes=[iq_])
                        sc.dma("sp", q_.t[:, :, :], QT[512:1024, tcol].rearrange("(h d) t -> d h t", d=64), q_, reads=[dQT], writes=[q_])
                        sc.dma("sp", g_.t[:, :, :], GT[512:1024, tcol].rearrange("(h d) t -> d h t", d=64), g_, reads=[dGT], writes=[g_])
                        def sc_mm(c):
                            k0 = 4 + 4 * c
                            w = min(4, Tq + 1 - k0) * 128
                            for hh in range(4):
                                ps = PS[hh]
                                r_ = rr[4 * (c % 2) + hh]
                                sc.op("pe", lambda e: e.matmul(ps.t[:, :w], lhsT=iq_.t[64:96, hh, :], rhs=kb.t[64:96, 0, k0 * 128:k0 * 128 + w],
                                                               start=True, stop=True), reads=[iq_, kb], writes=[ps])
                                sc.op("act", lambda e: e.activation(out=r_.t[:, :w], in_=ps.t[:, :w], func=AF.Relu), reads=[ps], writes=[r_])
                                sc.op("act", lambda e: e.activation(out=r_.t[:, :w], in_=r_.t[:, :w], func=AF.Copy,
                                                                    scale=iwall.t[:, si * 4 + ql, hh:hh + 1]), reads=[r_, iwall], writes=[r_])

                        def acc_mm(c):
                            k0 = 4 + 4 * c
                            w = min(4, Tq + 1 - k0) * 128
                            kc_ = kbc[kbi[0] % 4]; kbi[0] += 1
                            sc.dma("sp", kc_.t[:, :w], kbrow[0:1, k0 * 128:k0 * 128 + w].partition_broadcast(128)[:, 0, :], kc_, writes=[kc_])
                            dst = SCR.t[:, (k0 - 4) * 128:(k0 - 4) * 128 + w]
                            for hh in range(4):
                                r_ = rr[4 * (c % 2) + hh]
                                o_ap = dst if hh == 3 else kc_.t[:, :w]
                                if hh == 3:
                                    sc.op("pool", lambda e: e.tensor_tensor(out=dst, in0=kc_.t[:, :w], in1=r_.t[:, :w], op=ALU.add),
                                          reads=[kc_, r_], awrites=[SCR])
                                else:
                                    sc.op("pool", lambda e: e.tensor_tensor(out=kc_.t[:, :w], in0=kc_.t[:, :w], in1=r_.t[:, :w], op=ALU.add),
                                          reads=[kc_, r_], writes=[kc_])
                        plan = _topk_plan(N)
                        if plan is not None:
                            cs, R = plan
                            nck = (N + cs - 1) // cs
                            ncand = nck * R * 8
                            assert ncand <= NCAND and cs <= 1024, (N, plan)

                        def level1(j):
                            a0 = j * cs; a1 = min(N, a0 + cs)
                            srcap, srcbuf = SCR.t[:, a0:a1], SCR
                            for r in range(R):
                                co = (j * R + r) * 8
                                sc.op("dve", lambda e: e.max(out=cand[0].t[:, co:co + 8], in_=srcap), reads=[srcbuf], awrites=[cand[0]])
                                if r + 1 < R:
                                    nb = wk[r % 2]
                                    sc.op("dve", lambda e: e.match_replace(out=nb.t[:, :a1 - a0], in_to_replace=cand[0].t[:, co:co + 8],
                                                                            in_values=srcap, imm_value=-3.0e38),
                                          reads=[cand[0], srcbuf], writes=[nb])
                                    srcap, srcbuf = nb.t[:, :a1 - a0], nb
                        nextj = 0
                        sc_mm(0)
                        for c in range(nch):
                            if c + 1 < nch:
                                sc_mm(c + 1)
                            acc_mm(c)
                            if c == nch - 1:
                                sc.op("dve", lambda e: e.tensor_tensor(out=SCR.t[:, N - 128:N], in0=SCR.t[:, N - 128:N], in1=CAUS, op=ALU.add),
                                      reads=[SCR, C32], writes=[SCR])
                                cover = N
                            else:
                                cover = min(N - 128, (c + 1) * 512)
                            if plan is not None:
                                while nextj < nck and min(N, (nextj + 1) * cs) <= cover:
                                    level1(nextj); nextj += 1
                        if plan is None:
                            assert N <= 1024
                            cur, curb = SCR.t[:, :N], SCR
                            bufs2 = [(wk[0].t[:, :N], wk[0]), (wk[1].t[:, :N], wk[1])]
                        else:
                            assert nextj == nck
                            cur, curb = cand[0].t[:, :ncand], cand[0]
                            bufs2 = [(cand[1].t[:, :ncand], cand[1]), (cand[0].t[:, :ncand], cand[0])]
                        for r in range(33):
                            sc.op("dve", lambda e: e.max(out=m8.t[:, r * 8:(r + 1) * 8], in_=cur), reads=[curb], writes=[m8])
                            if r < 32:
                                nxt, nxtb = bufs2[r % 2]
                                sc.op("dve", lambda e: e.match_replace(out=nxt, in_to_replace=m8.t[:, r * 8:(r + 1) * 8], in_values=cur,
                                                                        imm_value=-3.0e38), reads=[m8, curb], writes=[nxtb])
                                cur, curb = nxt, nxtb
                        sc.op("dve", lambda e: e.tensor_scalar(out=thr.t[:, 1:2], in0=m8.t[:, 255:256], scalar1=0.5, scalar2=None, op0=ALU.mult),
                              reads=[m8], writes=[thr])
                        sc.op("dve", lambda e: e.scalar_tensor_tensor(out=thr.t[:, 0:1], in0=m8.t[:, 256:257], scalar=0.5, in1=thr.t[:, 1:2],
                                                                      op0=ALU.mult, op1=ALU.add), reads=[m8, thr], writes=[thr])

                    def stage2(v):
                        Tq, nkt, N, nch, iq_, q_, g_, o_, tcol, si, ql = (v[k] for k in ("Tq", "nkt", "N", "nch", "iq_", "q_", "g_", "o_", "tcol", "si", "ql"))
                        sc.op("dve", lambda e: e.tensor_scalar(out=dthr.t[:, :], in0=IDF, scalar1=thr.t[:, 0:1], scalar2=None, op0=ALU.mult),
                              reads=[thr, C32], writes=[dthr])
                        sc.op("pe", lambda e: e.matmul(PS[6].t[:, 0:128], lhsT=ONESF, rhs=dthr.t[:, :], start=True, stop=True),
                              reads=[dthr, C32], writes=[PS[6]])
                        sc.op("dve", lambda e: e.tensor_copy(out=thrbc.t[:, :], in_=PS[6].t[:, 0:128]), reads=[PS[6]], writes=[thrbc])
                        for c in range(nch):
                            k0 = 4 + 4 * c
                            nt_ = min(4, Tq + 1 - k0)
                            ps = PS[c % 2]
                            for t_ in range(nt_):
                                sc.op("pe", lambda e: e.transpose(out=ps.t[:, t_ * 128:(t_ + 1) * 128],
                                                                  in_=SCR.t[:, (k0 - 4 + t_) * 128:(k0 - 3 + t_) * 128], identity=IDF),
                                      reads=[SCR, C32], writes=[ps])
                            sc.op("dve", lambda e: e.tensor_tensor(out=NS.t[:, k0:k0 + nt_, :],
                                                                   in0=ps.t[:, :nt_ * 128].rearrange("p (t q) -> p t q", t=nt_),
                                                                   in1=thrbc.t[:, :].unsqueeze(1).to_broadcast([128, nt_, 128]), op=ALU.is_lt),
                                  reads=[ps, thrbc], awrites=[NS])

                    def stage3(v):
                        Tq, nkt, N, nch, iq_, q_, g_, o_, tcol, si, ql = (v[k] for k in ("Tq", "nkt", "N", "nch", "iq_", "q_", "g_", "o_", "tcol", "si", "ql"))
                        units = [(kt, kv) for kt in range(4, Tq + 1) for kv in range(2)]
                        vcur = {}

                        def qk(u, pidx):
                            kt, kv = u
                            ps = PS[pidx % 4]
                            dd = Tq - kt
                            sc.op("pe", lambda e: e.matmul(ps.t[:, :].rearrange("p (h q) -> p h q", h=4), lhsT=kb.t[:64, kv, kt * 128:(kt + 1) * 128],
                                                           rhs=q_.t[:, 4 * kv:4 * kv + 4, :], start=True, stop=False), reads=[kb, q_], writes=[ps])
                            sc.op("pe", lambda e: e.matmul(ps.t[:, :].rearrange("p (h q) -> p h q", h=4), lhsT=INEG,
                                                           rhs=NS.t[:, kt, :].unsqueeze(1).to_broadcast([128, 4, 128]), start=False, stop=(dd >= 5)),
                                  reads=[CBF, NS], writes=[ps])
                            if dd < 5:
                                sc.op("pe", lambda e: e.matmul(ps.t[:, :].rearrange("p (h q) -> p h q", h=4), lhsT=I8,
                                                               rhs=BMB.t[:, dd, 4 * kv:4 * kv + 4, :], start=False, stop=True),
                                      reads=[CBF, BMB], writes=[ps])
                        def load_chunk(k0):
                            vb_ = vbc[vci_box[0] % 4]; vci_box[0] += 1
                            ntl = min(4, Tq + 1 - k0)
                            sc.dma("sp", vb_.t[:, :ntl, :], VV[k0 * 128:(k0 + ntl) * 128, 128:384].rearrange("(t p) x -> p t x", p=128), vb_,
                                   reads=[dVV], writes=[vb_])
                            return vb_
                        qk(units[0], 0)
                        for n_, (kt, kv) in enumerate(units):
                            if n_ + 1 < len(units):
                                qk(units[n_ + 1], n_ + 1)
                            if (kt - 4) % 4 == 0 and kv == 0:
                                if kt == 4:
                                    vcur[4] = load_chunk(4)
                                if kt + 4 <= Tq:
                                    vcur[kt + 4] = load_chunk(kt + 4)
                                vcur["b"] = vcur[kt]; vcur["k0"] = kt
                            vb_ = vcur["b"]
                            ps = PS[n_ % 4]; p_ = pt[n_ % 4]
                            acc = PS[4 + kv]
                            sc.op("act", lambda e: e.activation(out=p_.t[:, :], in_=ps.t[:, :], func=AF.Exp, scale=0.125,
                                                                bias=KBIAS[:, kt:kt + 1]), reads=[ps, C32], writes=[p_])
                            sc.op("pe", lambda e: e.matmul(acc.t[:, :], lhsT=vb_.t[:, kt - vcur["k0"], kv * 128:(kv + 1) * 128], rhs=p_.t[:, :],
                                                           start=(kt == 4), stop=(kt == Tq)), reads=[vb_, p_], writes=[acc])
                        for kv in range(2):
                            attn_epilogue(PS[4 + kv], T1, PS[6], tmps, g_.t[:, 4 * kv:4 * kv + 4, :], g_, o_.t[:, 4 * kv:4 * kv + 4, :], o_)
                        sc.dma("pool", BR[512:1024, tcol].rearrange("(h d) t -> d h t", d=64), o_.t[:, :, :], o_, reads=[o_], awrites=[dBR])
                    QL = [qvars(si, g, ql, i4 * 4 + ql) for i4, (si, g) in enumerate(enumerate(own)) for ql in range(4)]
                    stage1(QL[0])
                    for n_q, v in enumerate(QL):
                        stage2(v)
                        if n_q + 1 < len(QL):
                            stage1(QL[n_q + 1])
                        stage3(v)
                    sc.barrier()

                with ExitStack() as st:
                    wb = sb(st, "wb", [128, 12, D], BF16)
                    wo = sb(st, "wo", [128, 8, D], BF16)
                    wst = [sb(st, "wst3_%d" % i, [128, 4, 512]) for i in range(2)]
                    br = [sb(st, "br%d" % i, [128, 12, 512], BF16) for i in range(2)]
                    mg = [sb(st, "mg%d" % i, [128, 3, 512], BF16) for i in range(2)]
                    tm = [sb(st, "tm%d" % i, [128, 512]) for i in range(3)]
                    mgd = [sb(st, "mgd%d" % i, [128, 8, 512], BF16) for i in range(2)]
                    xt = [sb(st, "x3_%d" % i, [128, D]) for i in range(2)]
                    xo = [sb(st, "xo%d" % i, [128, D]) for i in range(2)]
                    ci = 0
                    wbv = w_br[l].rearrange("n (c p) d -> p (n c) d", p=128)
                    wov = w_out[l].rearrange("(c p) d -> p c d", p=128)
                    for (srcv, dstb, nchunk) in [(wbv, wb, 12), (wov, wo, 8)]:
                        for c4 in range(0, nchunk, 4):
                            for d0 in (0, 512):
                                s_ = wst[ci % 2]
                                sc.dma("sp", s_.t[:, :, :], srcv[:, c4:c4 + 4, d0:d0 + 512], s_, writes=[s_])
                                en = ["dve", "pool"][ci % 2]; ci += 1
                                sc.op(en, lambda e: e.tensor_copy(out=dstb.t[:, c4:c4 + 4, d0:d0 + 512], in_=s_.t[:, :, :]), reads=[s_], awrites=[dstb])
                    for si, g in enumerate(own):
                        tq = slice(si * 512, (si + 1) * 512)
                        b_, md = br[si % 2], mgd[si % 2]
                        sc.dma("sp", b_.t[:, :, :], BR[:, tq].rearrange("(c p) t -> p c t", p=128), b_, reads=[dBR], writes=[b_])
                        for dc in range(8):
                            m_ = mg[dc % 2]
                            sc.dma("sp", m_.t[:, :, :], MT[:, tq].rearrange("(n c p) t -> p n c t", p=128, n=3)[:, :, dc, :], m_, reads=[dMT], writes=[m_])
                            for n in range(3):
                                ps = PS[n]
                                for c in range(4):
                                    sc.op("pe", lambda e: e.matmul(ps.t[:, :], lhsT=wb.t[:, 4 * n + c, dc * 128:(dc + 1) * 128], rhs=b_.t[:, 4 * n + c, :],
                                                                   start=(c == 0), stop=(c == 3)), reads=[wb, b_], writes=[ps])
                                sc.op("dve", lambda e: e.tensor_tensor(out=tm[n].t[:, :], in0=ps.t[:, :], in1=m_.t[:, n, :], op=ALU.mult),
                                      reads=[ps, m_], writes=[tm[n]])
                            sc.op("pool", lambda e: e.tensor_tensor(out=tm[0].t[:, :], in0=tm[0].t[:, :], in1=tm[1].t[:, :], op=ALU.add),
                                  reads=[tm[0], tm[1]], writes=[tm[0]])
                            sc.op("pool", lambda e: e.tensor_tensor(out=md.t[:, dc, :], in0=tm[0].t[:, :], in1=tm[2].t[:, :], op=ALU.add),
                                  reads=[tm[0], tm[2]], awrites=[md])
                        for tt in range(4):
                            tile = 4 * g + tt
                            x_, xo_ = xt[tt % 2], xo[tt % 2]
                            sc.dma("sp", x_.t[:, :], xsrc[tile * 128:(tile + 1) * 128, :], x_, reads=([dXS] if dXS else []), writes=[x_])
                            for e2 in range(2):
                                ps = PS[4 + e2]
                                for dc in range(8):
                                    sc.op("pe", lambda e: e.matmul(ps.t[:, :], lhsT=md.t[:, dc, tt * 128:(tt + 1) * 128], rhs=wo.t[:, dc, e2 * 512:(e2 + 1) * 512],
                                                                   start=(dc == 0), stop=(dc == 7)), reads=[md, wo], writes=[ps])
                                sc.op("dve", lambda e: e.tensor_tensor(out=xo_.t[:, e2 * 512:(e2 + 1) * 512], in0=ps.t[:, :], in1=x_.t[:, e2 * 512:(e2 + 1) * 512],
                                                                       op=ALU.add), reads=[ps, x_], awrites=[xo_])
                            if l < depth - 1:
                                sc.dma("pool", X1[tile * 128:(tile + 1) * 128, :], xo_.t[:, :], xo_, reads=[xo_], awrites=[dX1])
                            else:
                                sc.dma("pool", y[(si * 4 + tt) * 128:(si * 4 + tt + 1) * 128, :], xo_.t[:, :], xo_, reads=[xo_], awrites=[dY])
                    sc.barrier()
    return nc, dict(NG=NG, SP=SP, NT=NT, NOUT=NOUT, NC32=NC32, NCBF=NCBF, ninst=sc.ninst, nsem=sc.nsem)


def _t5_bucket_np(delta):
    n = np.maximum(delta, 0)
    nf = np.maximum(n, 1).astype(np.float32)
    large = 16 + (np.log(nf / np.float32(16)) / np.float32(math.log(512 / 16)) * np.float32(16)).astype(np.int32)
    large = np.minimum(large, 31)
    return np.where(n < 16, n, large)


def _host_consts(S, j):
    NGP = S // 512 + 1
    NT = NGP * 4
    pad = 1024 - 512 * j
    k = np.arange(128)[:, None]
    q = np.arange(128)[None, :]
    idf = np.eye(128, dtype=np.float32)
    ones = np.ones((128, 128), np.float32)
    bd = np.zeros((128, 128), np.float32); bd[:64, :64] = 1 / 64; bd[64:, 64:] = 1 / 64
    e0 = np.zeros((128, 128), np.float32); e0[0, :] = 1
    sh = np.zeros((128, 64), np.float32); sh[64 + np.arange(64), np.arange(64)] = 1
    caus = np.where(q > k, np.float32(-1e30), np.float32(0)).astype(np.float32)
    kbias = np.zeros((128, NT), np.float32); kbias[:, :pad // 128] = -30000.0
    kpos = 512 + np.arange(512)
    kpad = np.tile(np.where(kpos < pad, np.float32(-1e30), np.float32(0))[None, :], (128, 1)).astype(np.float32)
    c32 = np.concatenate([idf, ones, bd, e0, sh, caus, kbias, kpad], axis=1).astype(np.float32)
    allpos = np.arange(NGP * 512)
    kbrow = np.where(allpos < pad, np.float32(-1e30), (-(allpos.astype(np.float64)) * 1e-30).astype(np.float32))[None, :].astype(np.float32)
    i8 = 8.0 * idf
    ineg = -30000.0 * idf
    ma0 = np.where(k <= q, 0.0, NEGM)
    ma1 = np.where(k > q, 0.0, NEGM)
    cms = []
    qq = np.arange(512)[None, :]
    for i in range(4):
        cms.append(np.where(128 * i + k <= qq, 0.0, NEGM))
    cbf = np.concatenate([i8, ineg, ma0, ma1] + cms + [idf], axis=1).astype(ml_dtypes.bfloat16)
    return c32, cbf, kbrow


def _t5_tiles(rel_bias_cols, dds):
    k = np.arange(128)[:, None]
    q = np.arange(128)[None, :]
    out = []
    for dd in dds:
        if dd is None:
            b = np.full((128, 128), 31, np.int64)
        else:
            b = _t5_bucket_np(dd * 128 + q - k)
        t = rel_bias_cols[b]
        out.append(np.ascontiguousarray(t.transpose(0, 2, 1)).reshape(128, 1024))
    return np.stack(out).astype(np.float32)


def make_in_maps(S, x, norm_gain, w_in, b_forget, qk_gain, sinks, w_branch, w_out, rel_bias):
    B = x.shape[0]
    depth = w_in.shape[0]
    SP = S + 512
    f = lambda a: np.ascontiguousarray(a, dtype=np.float32)
    gainbc = f(np.broadcast_to(norm_gain[:, None, :], (depth, 128, D)))
    qkg = np.zeros((depth, 128, 8), np.float32)
    for n in range(3):
        for r in range(2):
            qkg[:, :, 2 * n + r] = np.tile(qk_gain[:, n, r, :], (1, 2))
    bfc = np.zeros((depth, 128, 1), np.float32); bfc[:, :8, 0] = b_forget
    sinkrep = f(np.broadcast_to(np.repeat(sinks, 128, axis=1)[:, None, :], (depth, 128, 1024)))
    t5a = _t5_tiles(np.asarray(rel_bias)[:, :8], [0, 1])
    t5b = _t5_tiles(np.asarray(rel_bias)[:, 8:], [0, 1, 2, 3, 4, None])
    consts = [_host_consts(S, j) for j in range(2)]
    maps = []
    for b in range(B):
        for j in range(2):
            pad = 1024 - 512 * j
            ntok = SP - pad
            xs_ = np.zeros((SP, D), np.float32)
            xs_[pad:pad + ntok] = x[b, :ntok]
            maps.append({"xs": xs_, "w_in": f(w_in), "w_br": f(w_branch), "w_out": f(w_out), "gainbc": gainbc, "qkg": qkg,
                         "bfc": bfc, "sinkrep": sinkrep, "t5a": t5a, "t5b": t5b, "c32": consts[j][0], "cbf": consts[j][1], "kbrow": consts[j][2]})
    return maps


def assemble(S, B, results):
    NG = S // 512
    out = np.zeros((B, S, D), np.float32)
    for b in range(B):
        for j in range(2):
            yv = np.asarray(results[2 * b + j]["y"])
            for si in range(NG // 2):
                gg = 2 * si + j
                out[b, gg * 512:(gg + 1) * 512] = yv[si * 512:(si + 1) * 512]
    return out


_CACHE = {}


def kernel(x, norm_gain, w_in, b_forget, qk_gain, sinks, w_branch, w_out, rel_bias):
    x = np.asarray(x, np.float32)
    B, S, _ = x.shape
    args = [np.asarray(a, np.float32) for a in (norm_gain, w_in, b_forget, qk_gain, sinks, w_branch, w_out, rel_bias)]
    if S not in _CACHE:
        _CACHE[S] = build_program(S, depth=args[1].shape[0])
    nc, info = _CACHE[S]
    maps = make_in_maps(S, x, *args)
    res = run_bass_kernel_spmd(nc, maps, core_ids=list(range(len(maps))))
    return assemble(S, B, res.results)
```

```python
import math
import numpy as np
import ml_dtypes
from contextlib import ExitStack
import concourse.bass as bass
import concourse.mybir as mybir
from concourse.bass_utils import run_bass_kernel_spmd

F32 = mybir.dt.float32
BF16 = mybir.dt.bfloat16
AF = mybir.ActivationFunctionType
ALU = mybir.AluOpType

D = 1024
INC = 7852
EPS = 1e-6
NEGM = -3750.0
TOPK = 256


class Buf:
    __slots__ = ("name", "t", "w", "r", "ds")

    def __init__(self, name, t=None):
        self.name, self.t, self.w, self.r, self.ds = name, t, {}, {}, {}


class Sched:
    def __init__(self, nc, es):
        self.nc, self.es = nc, es
        self.eng = {"pe": nc.tensor, "act": nc.scalar, "dve": nc.vector, "pool": nc.gpsimd, "sp": nc.sync}
        self.psem = {k: es.enter_context(nc.semaphore("p_" + k)) for k in ("pe", "act", "dve", "pool")}
        self.pcnt = {k: 0 for k in self.psem}
        self.waited = {k: {} for k in self.eng}
        self.bufs = []
        self.ninst = 0
        self.sempool = {"sp": [], "pool": []}
        self.nsem = 4

    def buf(self, name, t=None):
        b = Buf(name, t)
        self.bufs.append(b)
        return b

    def _wait(self, e, deps):
        for sid, (sem, val) in deps.items():
            if val <= 0 or self.waited[e].get(sid, 0) >= val:
                continue
            if e == "pe" and sem is self.psem["pe"]:
                continue
            self.eng[e].wait_ge(sem, val)
            self.waited[e][sid] = val
            self.ninst += 1

    @staticmethod
    def _merge(deps, d):
        for sid, tok in d.items():
            if sid not in deps or deps[sid][1] < tok[1]:
                deps[sid] = tok

    def _deps(self, reads, writes, awrites):
        deps = {}
        for b in reads:
            self._merge(deps, b.w)
        for b in writes:
            self._merge(deps, b.w)
            self._merge(deps, b.r)
        for b in awrites:
            self._merge(deps, b.r)
        return deps

    def _post(self, sem, val, reads, writes, awrites):
        tok = (sem, val)
        for b in reads:
            b.r[id(sem)] = tok
        for b in writes:
            b.w = {id(sem): tok}
            b.r = {}
        for b in awrites:
            b.w[id(sem)] = tok

    def op(self, e, fn, reads=(), writes=(), awrites=()):
        self._wait(e, self._deps(reads, writes, awrites))
        inst = fn(self.eng[e])
        self.pcnt[e] += 1
        inst.then_inc(self.psem[e], 1)
        self.ninst += 1
        self._post(self.psem[e], self.pcnt[e], reads, writes, awrites)
        return inst

    def dma(self, q, out, in_, owner, reads=(), writes=(), awrites=()):
        if q not in owner.ds:
            if self.sempool[q]:
                owner.ds[q] = list(self.sempool[q].pop())
            else:
                owner.ds[q] = [self.es.enter_context(self.nc.semaphore("d%s_%s" % (q, owner.name))), 0]
                self.nsem += 1
        d = owner.ds[q]
        deps = self._deps(reads, writes, awrites)
        self._merge(deps, {id(d[0]): (d[0], d[1])})
        self._wait(q, deps)
        inst = self.eng[q].dma_start(out=out, in_=in_)
        d[1] += 16
        inst.then_inc(d[0], 16)
        self.ninst += 1
        self._post(d[0], d[1], reads, writes, awrites)
        return inst

    def release(self, b):
        for q, d in b.ds.items():
            self.sempool[q].append((d[0], d[1]))
        b.ds = {}
        if b in self.bufs:
            self.bufs.remove(b)

    def barrier(self):
        toks = {id(s): (s, self.pcnt[k]) for k, s in self.psem.items()}
        for b in self.bufs:
            for d in b.ds.values():
                toks[id(d[0])] = (d[0], d[1])
        for e in self.eng:
            self._wait(e, toks)
        for b in self.bufs:
            b.w, b.r = {}, {}


def _pois_tail(m, k):
    p = math.exp(-m)
    c = p
    for i in range(1, k + 1):
        p *= m / i
        c += p
    return max(0.0, 1.0 - c)


def _topk_plan(n):
    best = (64 * n if n <= 1024 else 10**12, None)
    for cs in (128, 256, 512, 1024):
        nchunk = (n + cs - 1) // cs
        if nchunk < 2:
            continue
        m = TOPK * cs / n
        for r in range(1, 17):
            if 8 * r > cs or 8 * r * nchunk < TOPK + 16:
                continue
            if _pois_tail(m, 8 * r) * nchunk < 1e-10:
                cost = (2 * r - 1) * n + 66 * nchunk * 8 * r + 150 * nchunk * (2 * r - 1)
                if cost < best[0]:
                    best = (cost, (cs, r))
                break
    if False:
        cand = None
        for cs in (128, 256, 512, 1024):
            nchunk = (n + cs - 1) // cs
            m = TOPK * cs / n
            for r in range(1, 17):
                if 8 * r > cs or 8 * r * nchunk < TOPK + 16:
                    continue
                if _pois_tail(m, 8 * r) * nchunk < 1e-10:
                    cost = (2 * r - 1) * n + 66 * nchunk * 8 * r
                    if cand is None or cost < cand[0]:
                        cand = (cost, (cs, r))
                    break
        return cand[1]
    return best[1]


def build_program(S, depth=2, dbg=False):
    NG = S // 512
    NGP = NG + 1
    SP = NGP * 512
    NT = NGP * 4
    own_by_layer = [list(range(1, NG + 1)) if l < depth - 1 else list(range(2, NG + 1, 2)) for l in range(depth)]
    TOKQ = NG * 512
    NOUT = len(own_by_layer[-1])

    nc = bass.Bass("TRN2", target_bir_lowering=False)
    dt_in = lambda n, s, d=F32: nc.dram_tensor(n, list(s), d, kind="ExternalInput").ap()
    dt_sc = lambda n, s, d=BF16: nc.dram_tensor(n, list(s), d, kind="Internal").ap()
    xs = dt_in("xs", [SP, D])
    w_in = dt_in("w_in", [depth, D, INC])
    w_br = dt_in("w_br", [depth, 3, 512, D])
    w_out = dt_in("w_out", [depth, D, D])
    gainbc = dt_in("gainbc", [depth, 128, D])
    qkg = dt_in("qkg", [depth, 128, 8])
    bfc = dt_in("bfc", [depth, 128, 1])
    sinkrep = dt_in("sinkrep", [depth, 128, 1024])
    t5a = dt_in("t5a", [2, 128, 1024])
    t5b = dt_in("t5b", [6, 128, 1024])
    NC32 = 128 * 4 + 64 + 128 + NT + 512
    c32 = dt_in("c32", [128, NC32])
    kbrow = dt_in("kbrow", [1, SP])
    NCBF = 128 * 5 + 4 * 512
    cbf = dt_in("cbf", [128, NCBF], BF16)
    y = nc.dram_tensor("y", [NOUT * 512, D], F32, kind="ExternalOutput").ap()

    QT = dt_sc("QT", [1664, TOKQ])
    KT = dt_sc("KT", [800, SP])
    VV = dt_sc("VV", [SP, 896])
    GT = dt_sc("GT", [1536, TOKQ])
    MT = dt_sc("MT", [3072, TOKQ])
    BR = dt_sc("BR", [1536, TOKQ])
    X1 = dt_sc("X1", [SP, D], F32)
    dbgo = {}

    es = ExitStack()
    with es:
        sc = Sched(nc, es)

        uid = [0]

        def sb(st, name, shape, dt=F32):
            uid[0] += 1
            name = "%s_%d" % (name, uid[0])
            b = sc.buf(name, st.enter_context(nc.sbuf_tensor(name, list(shape), dt)))
            if st is not es:
                st.callback(sc.release, b)
            return b

        def pst(st, name, shape, dt=F32):
            return sc.buf(name, st.enter_context(nc.psum_tensor(name, list(shape), dt)))

        dQT, dKT, dVV, dGT, dMT, dBR, dX1, dY = (sc.buf(n) for n in ("dQT", "dKT", "dVV", "dGT", "dMT", "dBR", "dX1", "dY"))

        C32 = sb(es, "C32", [128, NC32])
        CBF = sb(es, "CBF", [128, NCBF], BF16)
        o = 0
        IDF = C32.t[:, 0:128]; ONESF = C32.t[:, 128:256]; BDF = C32.t[:, 256:384]; E0F = C32.t[:, 384:512]
        SHF = C32.t[:, 512:576]; CAUS = C32.t[:, 576:704]
        KBIAS = C32.t[:, 704:704 + NT]; KPAD = C32.t[:, 704 + NT:704 + NT + 512]
        I8 = CBF.t[:, 0:128]; INEG = CBF.t[:, 128:256]; MA0 = CBF.t[:, 256:384]; MA1 = CBF.t[:, 384:512]
        CM = [CBF.t[:, 512 + 512 * i:512 + 512 * (i + 1)] for i in range(4)]
        IDB = CBF.t[:, 2560:2688]
        ONE8 = sb(es, "ONE8", [8, 512])
        EPSC = sb(es, "EPSC", [128, 1])
        PS = [pst(es, "ps%d" % i, [128, 512]) for i in range(7)]
        PSB = pst(es, "psb", [128, 1024], BF16)

        sc.dma("sp", C32.t[:, :], c32[:, :], C32, writes=[C32])
        sc.dma("sp", CBF.t[:, :], cbf[:, :], CBF, writes=[CBF])
        sc.op("pool", lambda e: e.memset(ONE8.t[:, :], 1.0), writes=[ONE8])
        sc.op("pool", lambda e: e.memset(EPSC.t[:, :], 1e-18), writes=[EPSC])
        sc.barrier()

        def build_bma(st):
            BMA = sb(st, "BMA", [128, 2, 8, 128], BF16)
            tmp = sb(st, "t5tmp", [128, 1024])
            for dd in range(2):
                sc.dma("sp", tmp.t[:, :], t5a[dd], tmp, writes=[tmp])
                msk = MA0 if dd == 0 else MA1
                sc.op("dve", lambda e: e.tensor_tensor(
                    out=BMA.t[:, dd], in0=tmp.t[:, :].rearrange("p (h q) -> p h q", h=8),
                    in1=msk.unsqueeze(1).to_broadcast([128, 8, 128]), op=ALU.add), reads=[tmp, CBF], awrites=[BMA])
            return BMA

        def build_bmb(st):
            BMB = sb(st, "BMB", [128, 5, 8, 128], BF16)
            with ExitStack() as st2:
                tmp = sb(st2, "t5tmpb", [128, 1024])
                tfar = sb(st2, "t5far", [128, 1024])
                sc.dma("sp", tfar.t[:, :], t5b[5], tfar, writes=[tfar])
                for dd in range(5):
                    sc.dma("sp", tmp.t[:, :], t5b[dd], tmp, writes=[tmp])
                    sc.op("dve", lambda e: e.tensor_tensor(out=tmp.t[:, :], in0=tmp.t[:, :], in1=tfar.t[:, :], op=ALU.subtract),
                          reads=[tfar, tmp], writes=[tmp])
                    if dd == 0:
                        sc.op("dve", lambda e: e.tensor_tensor(
                            out=BMB.t[:, 0], in0=tmp.t[:, :].rearrange("p (h q) -> p h q", h=8),
                            in1=MA0.unsqueeze(1).to_broadcast([128, 8, 128]), op=ALU.add), reads=[tmp, CBF], awrites=[BMB])
                    else:
                        sc.op("dve", lambda e: e.tensor_copy(
                            out=BMB.t[:, dd], in_=tmp.t[:, :].rearrange("p (h q) -> p h q", h=8)), reads=[tmp], awrites=[BMB])
                sc.barrier()
            return BMB

        def attn_epilogue(acc, T1, dps, tmps, gate_ap, gate_buf, out_ap, out_buf, sink_ap=None, sink_buf=None):
            d2, ln, rd, t2 = tmps
            sc.op("act", lambda e: e.activation(out=T1.t[:, :], in_=acc.t[:, :], func=AF.Copy), reads=[acc], writes=[T1])
            sc.op("pe", lambda e: e.matmul(dps.t[:64, :], lhsT=SHF, rhs=T1.t[:, :], start=True, stop=True),
                  reads=[T1, C32], writes=[dps])
            if sink_ap is not None:
                sc.op("dve", lambda e: e.tensor_tensor(out=d2.t[:64, :], in0=dps.t[:64, :], in1=sink_ap, op=ALU.add),
                      reads=[dps, sink_buf], writes=[d2])
                sc.op("act", lambda e: e.activation(out=ln.t[:64, :], in_=d2.t[:64, :], func=AF.Ln), reads=[d2], writes=[ln])
            else:
                sc.op("act", lambda e: e.activation(out=ln.t[:64, :], in_=dps.t[:64, :], func=AF.Ln, bias=EPSC.t[:64, 0:1]),
                      reads=[dps, EPSC], writes=[ln])
            sc.op("act", lambda e: e.activation(out=rd.t[:64, :], in_=ln.t[:64, :], func=AF.Exp, scale=-1.0),
                  reads=[ln], writes=[rd])
            sc.op("pool", lambda e: e.tensor_tensor(out=t2.t[:64, :], in0=T1.t[:64, :], in1=rd.t[:64, :], op=ALU.mult),
                  reads=[T1, rd], writes=[t2])
            sc.op("pool", lambda e: e.tensor_tensor(out=out_ap, in0=t2.t[:64, :], in1=gate_ap, op=ALU.mult),
                  reads=[t2, gate_buf], awrites=[out_buf])

        for l in range(depth):
            own = own_by_layer[l]
            nown = len(own)
            xsrc, dXS = (xs, None) if l == 0 else (X1, dX1)
            xdst, dXD = (X1, dX1) if l < depth - 1 else (y, dY)
            lst = ExitStack()
            with lst:
                ckey = sb(lst, "ckey", [128, NT, 8])
                crefbc = sb(lst, "crefbc", [128, NGP, 8])
                iwall = sb(lst, "iwall", [128, nown * 4, 4])
                qkgs = sb(lst, "qkgs", [128, 8])
                sc.dma("sp", qkgs.t[:, :], qkg[l], qkgs, writes=[qkgs])

                for (cbase, ccount, pname) in [(0, 4780, "main"), (4780, 3072, "merge")]:
                  with ExitStack() as st:
                    Wbf = sb(st, "Wbf", [128, 8, ccount], BF16)
                    wst = [sb(st, "wst%d" % i, [128, 8, 128]) for i in range(2)]
                    gbc = sb(st, "gbc", [128, D])
                    bfs = sb(st, "bfs", [128, 1])
                    xts = [sb(st, "xt%d" % i, [128, D]) for i in range(2)]
                    junk = sb(st, "junk", [128, D], BF16)
                    hb = [sb(st, "hb%d" % i, [128, D], BF16) for i in range(2)]
                    hT = [sb(st, "hT%d" % i, [128, 8, 512], BF16) for i in range(2)]
                    ss = [sb(st, "ss%d" % i, [128, 4]) for i in range(2)]
                    sq = sb(st, "sq", [128, 512]); lnb = sb(st, "lnb", [128, 512]); rsb = sb(st, "rsb", [128, 512])
                    stg = [sb(st, "stg%d" % i, [128, 512], BF16) for i in range(4)]
                    vst = [sb(st, "vst%d" % i, [128, 1024], BF16) for i in range(2)]
                    fz = sb(st, "fz", [8, 512]); fe = sb(st, "fe", [8, 512]); fsp = sb(st, "fsp", [8, 512])
                    cg = sb(st, "cg", [8, 512]); cprev = sb(st, "cprev", [8, 1])
                    sc.dma("sp", gbc.t[:, :], gainbc[l], gbc, writes=[gbc])
                    sc.dma("sp", bfs.t[:, :], bfc[l], bfs, writes=[bfs])
                    sc.op("dve", lambda e: e.memset(cprev.t[:, :], 0.0), writes=[cprev])
                    for v_ in vst:
                        sc.op("pool", lambda e: e.memset(v_.t[:, :], 1.0), writes=[v_])
                    wv = w_in[l].rearrange("(c p) n -> p c n", p=128)
                    ceng = ["dve", "pool", "act"]
                    for ci, c0 in enumerate(range(0, ccount, 128)):
                        cw = min(128, ccount - c0)
                        s_ = wst[ci % 2]
                        sc.dma("sp", s_.t[:, :, :cw], wv[:, :, cbase + c0:cbase + c0 + cw], s_, writes=[s_])
                        en = ceng[ci % 3]
                        if en == "act":
                            sc.op("act", lambda e: e.activation(out=Wbf.t[:, :, c0:c0 + cw], in_=s_.t[:, :, :cw], func=AF.Copy),
                                  reads=[s_], awrites=[Wbf])
                        else:
                            sc.op(en, lambda e: e.tensor_copy(out=Wbf.t[:, :, c0:c0 + cw], in_=s_.t[:, :, :cw]),
                                  reads=[s_], awrites=[Wbf])
                    stg_i = [0]
                    psr = [0]

                    def nextps():
                        psr[0] = (psr[0] + 1) % 4
                        return PS[psr[0]]

                    def store_fm(src_ap_fn, M, dram_ap, dbuf):
                        s_ = stg[stg_i[0] % 4]; stg_i[0] += 1
                        src_ap_fn(s_)
                        sc.dma("pool", dram_ap, s_.t[:M, :], s_, reads=[s_], awrites=[dbuf])

                    def fm_mm(ps, c0, M, h):
                        c0 = c0 - cbase
                        for c in range(8):
                            sc.op("pe", lambda e: e.matmul(ps.t[:M, :], lhsT=Wbf.t[:, c, c0:c0 + M], rhs=h.t[:, c, :],
                                                           start=(c == 0), stop=(c == 7)), reads=[Wbf, h], writes=[ps])

                    def ep_norm(ps, gcol, dram_ap, dbuf):
                        sc.op("act", lambda e: e.activation(out=sq.t[:, :], in_=ps.t[:, :], func=AF.Square), reads=[ps], writes=[sq])
                        sc.op("pe", lambda e: e.matmul(PS[4].t[:, :], lhsT=BDF, rhs=sq.t[:, :], start=True, stop=True),
                              reads=[sq, C32], writes=[PS[4]])
                        sc.op("dve", lambda e: e.tensor_scalar(out=lnb.t[:, :], in0=PS[4].t[:, :], scalar1=EPS, scalar2=None, op0=ALU.add),
                              reads=[PS[4]], writes=[lnb])
                        sc.op("act", lambda e: e.activation(out=rsb.t[:, :], in_=lnb.t[:, :], func=AF.Ln), reads=[lnb], writes=[rsb])
                        sc.op("act", lambda e: e.activation(out=lnb.t[:, :], in_=rsb.t[:, :], func=AF.Exp, scale=-0.5), reads=[rsb], writes=[lnb])
                        store_fm(lambda s_: sc.op("dve", lambda e: e.scalar_tensor_tensor(
                            out=s_.t[:, :], in0=ps.t[:, :], scalar=qkgs.t[:, gcol:gcol + 1], in1=lnb.t[:, :], op0=ALU.mult, op1=ALU.mult),
                            reads=[ps, qkgs, lnb], writes=[s_]), 128, dram_ap, dbuf)

                    def ep_act(ps, M, func, dram_ap, dbuf):
                        store_fm(lambda s_: sc.op("act", lambda e: e.activation(out=s_.t[:M, :], in_=ps.t[:M, :], func=func),
                                                  reads=[ps], writes=[s_]), M, dram_ap, dbuf)

                    for g in range(1, NG + 1):
                        h = hT[g % 2]
                        isown = g in own
                        si = own.index(g) if isown else -1
                        if pname == "merge" and not isown:
                            continue
                        for tt in range(4):
                            tile = 4 * g + tt
                            xt = xts[tt % 2]; hbb = hb[tt % 2]; s4 = ss[tt % 2]
                            sc.dma("sp", xt.t[:, :], xsrc[tile * 128:(tile + 1) * 128, :], xt, reads=([dXS] if dXS else []), writes=[xt])
                            sc.op("act", lambda e: e.activation(out=junk.t[:, :], in_=xt.t[:, :], func=AF.Square, accum_out=s4.t[:, 0:1]),
                                  reads=[xt], writes=[junk, s4])
                            sc.op("dve", lambda e: e.tensor_scalar(out=s4.t[:, 1:2], in0=s4.t[:, 0:1], scalar1=1.0 / D, scalar2=EPS,
                                                                    op0=ALU.mult, op1=ALU.add), reads=[s4], writes=[s4])
                            sc.op("act", lambda e: e.activation(out=s4.t[:, 2:3], in_=s4.t[:, 1:2], func=AF.Ln), reads=[s4], writes=[s4])
                            sc.op("act", lambda e: e.activation(out=s4.t[:, 3:4], in_=s4.t[:, 2:3], func=AF.Exp, scale=-0.5), reads=[s4], writes=[s4])
                            sc.op("dve", lambda e: e.scalar_tensor_tensor(out=hbb.t[:, :], in0=xt.t[:, :], scalar=s4.t[:, 3:4], in1=gbc.t[:, :],
                                                                           op0=ALU.mult, op1=ALU.mult), reads=[xt, s4, gbc], writes=[hbb])
                            for c in range(8):
                                sc.op("pe", lambda e: e.transpose(out=PSB.t[:, c * 128:(c + 1) * 128], in_=hbb.t[:, c * 128:(c + 1) * 128],
                                                                  identity=IDB), reads=[hbb, CBF], writes=[PSB])
                            sc.op("dve", lambda e: e.tensor_copy(out=h.t[:, :, tt * 128:(tt + 1) * 128],
                                                                 in_=PSB.t[:, :].rearrange("p (c t) -> p c t", c=8)), reads=[PSB], awrites=[h])
                        tk = slice(g * 512, (g + 1) * 512)
                        if pname == "merge":
                            tq = slice(si * 512, (si + 1) * 512)
                            for i in range(24):
                                ps = nextps(); fm_mm(ps, 4780 + 128 * i, 128, h); ep_act(ps, 128, AF.Sigmoid, MT[128 * i:128 * (i + 1), tq], dMT)
                            continue
                        for (c0, gcol, r0) in [(512, 1, 0), (1792, 3, 128)] + [(3236 + 128 * i, 5, 256 + 128 * i) for i in range(4)]:
                            ps = nextps(); fm_mm(ps, c0, 128, h); ep_norm(ps, gcol, KT[r0:r0 + 128, tk], dKT)
                        ps = nextps(); fm_mm(ps, 2176, 32, h); ep_act(ps, 32, AF.Copy, KT[768:800, tk], dKT)
                        ps = nextps(); fm_mm(ps, 4260, 8, h)
                        sc.op("dve", lambda e: e.tensor_scalar(out=fz.t[:, :], in0=ps.t[:8, :], scalar1=bfs.t[:8, 0:1], scalar2=None, op0=ALU.add),
                              reads=[ps, bfs], writes=[fz])
                        sc.op("act", lambda e: e.activation(out=fe.t[:, :], in_=fz.t[:, :], func=AF.Exp, scale=-1.0), reads=[fz], writes=[fe])
                        sc.op("dve", lambda e: e.tensor_scalar(out=fz.t[:, :], in0=fe.t[:, :], scalar1=1.0, scalar2=None, op0=ALU.add),
                              reads=[fe], writes=[fz])
                        sc.op("act", lambda e: e.activation(out=fsp.t[:, :], in_=fz.t[:, :], func=AF.Ln), reads=[fz], writes=[fsp])
                        sc.op("dve", lambda e: e.tensor_tensor_scan(out=cg.t[:, :], data0=ONE8.t[:, :], data1=fsp.t[:, :], initial=cprev.t[:, 0:1],
                                                                     op0=ALU.mult, op1=ALU.subtract), reads=[ONE8, fsp, cprev], writes=[cg])
                        sc.op("dve", lambda e: e.tensor_copy(out=cprev.t[:, :], in_=cg.t[:, 511:512]), reads=[cg], writes=[cprev])
                        for tt in range(4):
                            sc.op("pe", lambda e: e.transpose(out=PS[5].t[:, tt * 8:(tt + 1) * 8], in_=cg.t[:8, tt * 128:(tt + 1) * 128],
                                                              identity=C32.t[:8, 0:8]), reads=[cg, C32], writes=[PS[5]])
                        sc.op("dve", lambda e: e.tensor_copy(out=ckey.t[:, 4 * g:4 * g + 4, :], in_=PS[5].t[:, 0:32].rearrange("p (t h) -> p t h", t=4)),
                              reads=[PS[5]], awrites=[ckey])
                        sc.op("pe", lambda e: e.matmul(PS[5].t[:, 32:40], lhsT=E0F, rhs=ckey.t[:, 4 * g, :], start=True, stop=True),
                              reads=[ckey, C32], writes=[PS[5]])
                        sc.op("dve", lambda e: e.tensor_copy(out=crefbc.t[:, g, :], in_=PS[5].t[:, 32:40]), reads=[PS[5]], awrites=[crefbc])
                        for tt in range(4):
                            tile = 4 * g + tt
                            vs_ = vst[tt % 2]
                            for (c0, n, pb, o0) in [(640, 128, PS[5], 0), (1920, 128, PS[5], 128), (3748, 512, PS[6], 0)]:
                                for c in range(8):
                                    sc.op("pe", lambda e: e.matmul(pb.t[:, o0:o0 + n], lhsT=h.t[:, c, tt * 128:(tt + 1) * 128],
                                                                   rhs=Wbf.t[:, c, c0 - cbase:c0 - cbase + n], start=(c == 0), stop=(c == 7)),
                                          reads=[Wbf, h], writes=[pb])
                            sc.op("dve", lambda e: e.tensor_copy(out=vs_.t[:, 0:128], in_=PS[5].t[:, 0:128]), reads=[PS[5]], awrites=[vs_])
                            sc.op("dve", lambda e: e.tensor_copy(out=vs_.t[:, 128:384].rearrange("p (h x) -> p h x", h=2)[:, :, 0:64],
                                                                 in_=PS[5].t[:, 128:256].rearrange("p (h d) -> p h d", h=2)), reads=[PS[5]], awrites=[vs_])
                            sc.op("act", lambda e: e.activation(out=vs_.t[:, 384:896], in_=PS[6].t[:, :], func=AF.Copy), reads=[PS[6]], awrites=[vs_])
                            sc.dma("pool", VV[tile * 128:(tile + 1) * 128, :], vs_.t[:, 0:896], vs_, reads=[vs_], awrites=[dVV])
                            if isown:
                                for c in range(8):
                                    sc.op("pe", lambda e: e.matmul(PS[5].t[:, 256:260], lhsT=h.t[:, c, tt * 128:(tt + 1) * 128],
                                                                   rhs=Wbf.t[:, c, 2208 - cbase:2212 - cbase], start=(c == 0), stop=(c == 7)),
                                          reads=[Wbf, h], writes=[PS[5]])
                                sc.op("dve", lambda e: e.tensor_copy(out=iwall.t[:, si * 4 + tt, :], in_=PS[5].t[:, 256:260]),
                                      reads=[PS[5]], awrites=[iwall])
                        if not isown:
                            continue
                        tq = slice(si * 512, (si + 1) * 512)
                        for (b0, gcol, r0) in [(0, 0, 0), (1280, 2, 512), (2724, 4, 1024)]:
                            for i in range(4):
                                ps = nextps(); fm_mm(ps, b0 + 128 * i, 128, h); ep_norm(ps, gcol, QT[r0 + 128 * i:r0 + 128 * (i + 1), tq], dQT)
                        ps = nextps(); fm_mm(ps, 2048, 128, h); ep_act(ps, 128, AF.Copy, QT[1536:1664, tq], dQT)
                        for (b0, r0) in [(768, 0), (2212, 512), (4268, 1024)]:
                            for i in range(4):
                                ps = nextps(); fm_mm(ps, b0 + 128 * i, 128, h); ep_act(ps, 128, AF.Silu, GT[r0 + 128 * i:r0 + 128 * (i + 1), tq], dGT)
                    sc.barrier()

                with ExitStack() as st:
                    BMA = build_bma(st)
                    sinkx = sb(st, "sinkx", [128, 1024])
                    sc.dma("sp", sinkx.t[:, :], sinkrep[l], sinkx, writes=[sinkx])
                    sc.op("act", lambda e: e.activation(out=sinkx.t[:, :], in_=sinkx.t[:, :], func=AF.Exp), reads=[sinkx], writes=[sinkx])
                    qa = [sb(st, "qa%d" % i, [64, 8, 512], BF16) for i in range(2)]
                    ga = [sb(st, "ga%d" % i, [64, 8, 512], BF16) for i in range(2)]
                    ka = [sb(st, "ka%d" % i, [64, 2, 640], BF16) for i in range(2)]
                    va = [sb(st, "va%d" % i, [128, 5, 2, 128], BF16) for i in range(2)]
                    oa = [sb(st, "oa%d" % i, [64, 8, 512], BF16) for i in range(2)]
                    pt = [sb(st, "pt%d" % i, [128, 512], BF16) for i in range(3)]
                    T1 = sb(st, "T1", [128, 512]); tmps = [sb(st, "ta%d" % i, [64, 512]) for i in range(4)]
                    for v_ in va:
                        sc.op("pool", lambda e, v_=v_: e.memset(v_.t[:, :, :, 64:128], 1.0), awrites=[v_])
                    pti = 0
                    for si, g in enumerate(own):
                        q_, g_, k_, v_, o_ = qa[si % 2], ga[si % 2], ka[si % 2], va[si % 2], oa[si % 2]
                        tq = slice(si * 512, (si + 1) * 512)
                        sc.dma("sp", q_.t[:, :, :], QT[0:512, tq].rearrange("(h d) t -> d h t", d=64), q_, reads=[dQT], writes=[q_])
                        sc.dma("sp", g_.t[:, :, :], GT[0:512, tq].rearrange("(h d) t -> d h t", d=64), g_, reads=[dGT], writes=[g_])
                        t0 = 4 * g - 1
                        f0 = 1 if g == 1 else 0
                        sc.dma("sp", k_.t[:, :, f0 * 128:], KT[0:128, (t0 + f0) * 128:(t0 + 5) * 128].rearrange("(h d) t -> d h t", d=64), k_,
                               reads=[dKT], writes=[k_])
                        for kv in range(2):
                            sc.dma("sp", v_.t[:, f0:, kv, 0:64], VV[(t0 + f0) * 128:(t0 + 5) * 128, 64 * kv:64 * (kv + 1)].rearrange("(t p) d -> p t d", p=128), v_,
                                   reads=[dVV], awrites=[v_])
                        units = []
                        for kv in range(2):
                            for ql in range(4):
                                kts = [ql, ql + 1] if not (g == 1 and ql == 0) else [ql + 1]
                                for n_, ktl in enumerate(kts):
                                    units.append((kv, ql, ktl, n_, len(kts)))

                        def qk_a(u, idx):
                            kv, ql, ktl, n_, nk_ = u
                            ps = PS[idx % 3]
                            dd = 1 if ktl == ql else 0
                            sc.op("pe", lambda e: e.matmul(ps.t[:, :].rearrange("p (h q) -> p h q", h=4), lhsT=k_.t[:, kv, ktl * 128:(ktl + 1) * 128],
                                                           rhs=q_.t[:, 4 * kv:4 * kv + 4, ql * 128:(ql + 1) * 128], start=True, stop=False),
                                  reads=[k_, q_], writes=[ps])
                            sc.op("pe", lambda e: e.matmul(ps.t[:, :].rearrange("p (h q) -> p h q", h=4), lhsT=I8,
                                                           rhs=BMA.t[:, dd, 4 * kv:4 * kv + 4, :], start=False, stop=True),
                                  reads=[CBF, BMA], writes=[ps])
                        qk_a(units[0], pti)
                        for i_, u in enumerate(units):
                            kv, ql, ktl, n_, nk_ = u
                            if i_ + 1 < len(units):
                                qk_a(units[i_ + 1], pti + i_ + 1)
                            ps = PS[(pti + i_) % 3]; p_ = pt[(pti + i_) % 3]
                            acc = PS[4 + (ql % 2)]
                            kt_abs = t0 + ktl
                            sc.op("act", lambda e: e.activation(out=p_.t[:, :], in_=ps.t[:, :], func=AF.Exp, scale=0.125,
                                                                bias=KBIAS[:, kt_abs:kt_abs + 1]), reads=[ps, C32], writes=[p_])
                            sc.op("pe", lambda e: e.matmul(acc.t[:, :], lhsT=v_.t[:, ktl, kv, :], rhs=p_.t[:, :],
                                                           start=(n_ == 0), stop=(n_ == nk_ - 1)), reads=[v_, p_], writes=[acc])
                            if n_ == nk_ - 1:
                                attn_epilogue(acc, T1, PS[6], tmps,
                                              g_.t[:, 4 * kv:4 * kv + 4, ql * 128:(ql + 1) * 128],
                                              g_, o_.t[:, 4 * kv:4 * kv + 4, ql * 128:(ql + 1) * 128], o_,
                                              sink_ap=sinkx.t[:64, kv * 512:(kv + 1) * 512], sink_buf=sinkx)
                        pti += len(units)
                        sc.dma("pool", BR[0:512, tq].rearrange("(h d) t -> d h t", d=64), o_.t[:, :, :], o_, reads=[o_], awrites=[dBR])
                    sc.barrier()

                with ExitStack() as st:
                    kc = [sb(st, "kc%d" % i, [64, SP], BF16) for i in range(2)]
                    vc = [sb(st, "vc%d" % i, [128, NT, 128], BF16) for i in range(2)]
                    qc = [sb(st, "qc%d" % i, [64, 512], BF16) for i in range(2)]
                    gc = [sb(st, "gc%d" % i, [64, 512], BF16) for i in range(2)]
                    oc = [sb(st, "oc%d" % i, [64, 512], BF16) for i in range(2)]
                    bias_h = [sb(st, "biash%d" % i, [128, NT]) for i in range(2)]
                    pt = [sb(st, "ptc%d" % i, [128, 512], BF16) for i in range(3)]
                    T1 = sb(st, "T1c", [128, 512]); tmps = [sb(st, "tc%d" % i, [64, 512]) for i in range(4)]
                    for v_ in vc:
                        sc.op("pool", lambda e, v_=v_: e.memset(v_.t[:, :, 64:128], 1.0), awrites=[v_])
                    it = 0
                    for hh in range(8):
                        k_, v_ = kc[hh % 2], vc[hh % 2]
                        sc.dma("sp", k_.t[:, 512:], KT[256 + 64 * hh:256 + 64 * (hh + 1), 512:], k_, reads=[dKT], writes=[k_])
                        sc.dma("sp", v_.t[:, 4:, 0:64], VV[512:, 384 + 64 * hh:384 + 64 * (hh + 1)].rearrange("(t p) d -> p t d", p=128), v_,
                               reads=[dVV], awrites=[v_])
                        for si, g in enumerate(own):
                            q_, g_, o_, bh = qc[it % 2], gc[it % 2], oc[it % 2], bias_h[it % 2]
                            acc = PS[4 + (it % 2)]
                            it += 1
                            tq = slice(si * 512, (si + 1) * 512)
                            sc.dma("sp", q_.t[:, :], QT[1024 + 64 * hh:1024 + 64 * (hh + 1), tq], q_, reads=[dQT], writes=[q_])
                            sc.dma("sp", g_.t[:, :], GT[1024 + 64 * hh:1024 + 64 * (hh + 1), tq], g_, reads=[dGT], writes=[g_])
                            nk = 4 * g + 4
                            sc.op("dve", lambda e: e.tensor_scalar(out=bh.t[:, 4:nk], in0=ckey.t[:, 4:nk, hh], scalar1=crefbc.t[:, g, hh:hh + 1],
                                                                    scalar2=-1.0, op0=ALU.subtract, op1=ALU.mult), reads=[ckey, crefbc], writes=[bh])
                            sc.op("dve", lambda e: e.tensor_tensor(out=bh.t[:, 4:nk], in0=bh.t[:, 4:nk], in1=KBIAS[:, 4:nk], op=ALU.add),
                                  reads=[bh, C32], writes=[bh])
                            kts = list(range(4, nk))

                            def qk(kt, pidx):
                                ps = PS[pidx % 3]
                                diag = kt >= 4 * g
                                sc.op("pe", lambda e: e.matmul(ps.t[:, :], lhsT=k_.t[:, kt * 128:(kt + 1) * 128], rhs=q_.t[:, :],
                                                               start=True, stop=not diag), reads=[k_, q_], writes=[ps])
                                if diag:
                                    sc.op("pe", lambda e: e.matmul(ps.t[:, :], lhsT=I8, rhs=CM[kt - 4 * g], start=False, stop=True),
                                          reads=[CBF], writes=[ps])
                            qk(kts[0], 0)
                            for n_, kt in enumerate(kts):
                                if n_ + 1 < len(kts):
                                    qk(kts[n_ + 1], n_ + 1)
                                ps = PS[n_ % 3]; p_ = pt[n_ % 3]
                                sc.op("act", lambda e: e.activation(out=p_.t[:, :], in_=ps.t[:, :], func=AF.Exp, scale=0.125,
                                                                    bias=bh.t[:, kt:kt + 1]), reads=[ps, bh], writes=[p_])
                                sc.op("pe", lambda e: e.matmul(acc.t[:, :], lhsT=v_.t[:, kt, :], rhs=p_.t[:, :],
                                                               start=(n_ == 0), stop=(n_ == len(kts) - 1)), reads=[v_, p_], writes=[acc])
                            attn_epilogue(acc, T1, PS[6], tmps, g_.t[:, :], g_, o_.t[:, :], o_)
                            sc.dma("pool", BR[1024 + 64 * hh:1024 + 64 * (hh + 1), tq], o_.t[:, :], o_, reads=[o_], awrites=[dBR])
                    sc.barrier()

                with ExitStack() as st:
                    BMB = build_bmb(st)
                    NKEY = (NT - 4) * 128
                    SCR = sb(st, "SCR", [128, NKEY])
                    NS = sb(st, "NS", [128, NT, 128], BF16)
                    kb = sb(st, "kb", [128, 2, SP], BF16)
                    vbc = [sb(st, "vbc%d" % i, [128, 4, 256], BF16) for i in range(4)]
                    iq = [sb(st, "iq%d" % i, [128, 4, 128], BF16) for i in range(2)]
                    qb = [sb(st, "qb%d" % i, [64, 8, 128], BF16) for i in range(2)]
                    gb = [sb(st, "gb%d" % i, [64, 8, 128], BF16) for i in range(2)]
                    ob = [sb(st, "ob%d" % i, [64, 8, 128], BF16) for i in range(2)]
                    rr = [sb(st, "rr%d" % i, [128, 512]) for i in range(8)]
                    kbc = [sb(st, "kbc%d" % i, [128, 512]) for i in range(4)]
                    kbi = [0]
                    dw = sb(st, "dw", [128, 4, 128])
                    pt = [sb(st, "ptb%d" % i, [128, 512], BF16) for i in range(4)]
                    T1 = sb(st, "T1b", [128, 512]); tmps = [sb(st, "tb%d" % i, [64, 512]) for i in range(4)]
                    NCAND = 1152
                    cand = [sb(st, "cand%d" % i, [128, NCAND]) for i in range(2)]
                    wk = [sb(st, "wk%d" % i, [128, 1024]) for i in range(2)]
                    m8 = sb(st, "m8", [128, 8 * 34])
                    thr = sb(st, "thr", [128, 2]); dthr = sb(st, "dthr", [128, 128]); thrbc = sb(st, "thrbc", [128, 128])
                    sc.dma("sp", kb.t[:64, :, 512:], KT[128:256, 512:].rearrange("(h d) t -> d h t", d=64), kb, reads=[dKT], awrites=[kb])
                    sc.dma("sp", kb.t[64:96, 0, 512:], KT[768:800, 512:], kb, reads=[dKT], awrites=[kb])
                    sc.barrier()
                    vci_box = [0]

                    def qvars(si, g, ql, qi):
                        Tq = 4 * g + ql
                        nkt = Tq - 3
                        return dict(Tq=Tq, nkt=nkt, N=nkt * 128, nch=(nkt + 3) // 4, iq_=iq[qi % 2], q_=qb[qi % 2], g_=gb[qi % 2], o_=ob[qi % 2],
                                    tcol=slice(si * 512 + ql * 128, si * 512 + (ql + 1) * 128), si=si, ql=ql)

                    def stage1(v):
                        Tq, nkt, N, nch, iq_, q_, g_, o_, tcol, si, ql = (v[k] for k in ("Tq", "nkt", "N", "nch", "iq_", "q_", "g_", "o_", "tcol", "si", "ql"))
                        sc.dma("sp", iq_.t[64:96, :, :], QT[1536:1664, tcol].rearrange("(h c) t -> c h t", c=32), iq_, reads=[dQT], writes=[iq_])
                        sc.dma("sp", q_.t[:, :, :], QT[512:1024, tcol].rearrange("(h d) t -> d h t", d=64), q_, reads=[dQT], writes=[q_])
                        sc.dma("sp", g_.t[:, :, :], GT[512:1024, tcol].rearrange("(h d) t -> d h t", d=64), g_, reads=[dGT], writes=[g_])
                        def sc_mm(c):
                            k0 = 4 + 4 * c
                            w = min(4, Tq + 1 - k0) * 128
                            for hh in range(4):
                                ps = PS[hh]
                                r_ = rr[4 * (c % 2) + hh]
                                sc.op("pe", lambda e: e.matmul(ps.t[:, :w], lhsT=iq_.t[64:96, hh, :], rhs=kb.t[64:96, 0, k0 * 128:k0 * 128 + w],
                                                               start=True, stop=True), reads=[iq_, kb], writes=[ps])
                                sc.op("act", lambda e: e.activation(out=r_.t[:, :w], in_=ps.t[:, :w], func=AF.Relu), reads=[ps], writes=[r_])
                                sc.op("act", lambda e: e.activation(out=r_.t[:, :w], in_=r_.t[:, :w], func=AF.Copy,
                                                                    scale=iwall.t[:, si * 4 + ql, hh:hh + 1]), reads=[r_, iwall], writes=[r_])

                        def acc_mm(c):
                            k0 = 4 + 4 * c
                            w = min(4, Tq + 1 - k0) * 128
                            kc_ = kbc[kbi[0] % 4]; kbi[0] += 1
                            sc.dma("sp", kc_.t[:, :w], kbrow[0:1, k0 * 128:k0 * 128 + w].partition_broadcast(128)[:, 0, :], kc_, writes=[kc_])
                            dst = SCR.t[:, (k0 - 4) * 128:(k0 - 4) * 128 + w]
                            for hh in range(4):
                                r_ = rr[4 * (c % 2) + hh]
                                o_ap = dst if hh == 3 else kc_.t[:, :w]
                                if hh == 3:
                                    sc.op("pool", lambda e: e.tensor_tensor(out=dst, in0=kc_.t[:, :w], in1=r_.t[:, :w], op=ALU.add),
                                          reads=[kc_, r_], awrites=[SCR])
                                else:
                                    sc.op("pool", lambda e: e.tensor_tensor(out=kc_.t[:, :w], in0=kc_.t[:, :w], in1=r_.t[:, :w], op=ALU.add),
                                          reads=[kc_, r_], writes=[kc_])
                        plan = _topk_plan(N)
                        if plan is not None:
                            cs, R = plan
                            nck = (N + cs - 1) // cs
                            ncand = nck * R * 8
                            assert ncand <= NCAND and cs <= 1024, (N, plan)

                        def level1(j):
                            a0 = j * cs; a1 = min(N, a0 + cs)
                            srcap, srcbuf = SCR.t[:, a0:a1], SCR
                            for r in range(R):
                                co = (j * R + r) * 8
                                sc.op("dve", lambda e: e.max(out=cand[0].t[:, co:co + 8], in_=srcap), reads=[srcbuf], awrites=[cand[0]])
                                if r + 1 < R:
                                    nb = wk[r % 2]
                                    sc.op("dve", lambda e: e.match_replace(out=nb.t[:, :a1 - a0], in_to_replace=cand[0].t[:, co:co + 8],
                                                                            in_values=srcap, imm_value=-3.0e38),
                                          reads=[cand[0], srcbuf], writes=[nb])
                                    srcap, srcbuf = nb.t[:, :a1 - a0], nb
                        nextj = 0
                        sc_mm(0)
                        for c in range(nch):
                            if c + 1 < nch:
                                sc_mm(c + 1)
                            acc_mm(c)
                            if c == nch - 1:
                                sc.op("dve", lambda e: e.tensor_tensor(out=SCR.t[:, N - 128:N], in0=SCR.t[:, N - 128:N], in1=CAUS, op=ALU.add),
                                      reads=[SCR, C32], writes=[SCR])
                                cover = N
                            else:
                                cover = min(N - 128, (c + 1) * 512)
                            if plan is not None:
                                while nextj < nck and min(N, (nextj + 1) * cs) <= cover:
                                    level1(nextj); nextj += 1
                        if plan is None:
                            assert N <= 1024
                            cur, curb = SCR.t[:, :N], SCR
                            bufs2 = [(wk[0].t[:, :N], wk[0]), (wk[1].t[:, :N], wk[1])]
                        else:
                            assert nextj == nck
                            cur, curb = cand[0].t[:, :ncand], cand[0]
                            bufs2 = [(cand[1].t[:, :ncand], cand[1]), (cand[0].t[:, :ncand], cand[0])]
                        for r in range(33):
                            sc.op("dve", lambda e: e.max(out=m8.t[:, r * 8:(r + 1) * 8], in_=cur), reads=[curb], writes=[m8])
                            if r < 32:
                                nxt, nxtb = bufs2[r % 2]
                                sc.op("dve", lambda e: e.match_replace(out=nxt, in_to_replace=m8.t[:, r * 8:(r + 1) * 8], in_values=cur,
                                                                        imm_value=-3.0e38), reads=[m8, curb], writes=[nxtb])
                                cur, curb = nxt, nxtb
                        sc.op("dve", lambda e: e.tensor_scalar(out=thr.t[:, 1:2], in0=m8.t[:, 255:256], scalar1=0.5, scalar2=None, op0=ALU.mult),
                              reads=[m8], writes=[thr])
                        sc.op("dve", lambda e: e.scalar_tensor_tensor(out=thr.t[:, 0:1], in0=m8.t[:, 256:257], scalar=0.5, in1=thr.t[:, 1:2],
                                                                      op0=ALU.mult, op1=ALU.add), reads=[m8, thr], writes=[thr])

                    def stage2(v):
                        Tq, nkt, N, nch, iq_, q_, g_, o_, tcol, si, ql = (v[k] for k in ("Tq", "nkt", "N", "nch", "iq_", "q_", "g_", "o_", "tcol", "si", "ql"))
                        sc.op("dve", lambda e: e.tensor_scalar(out=dthr.t[:, :], in0=IDF, scalar1=thr.t[:, 0:1], scalar2=None, op0=ALU.mult),
                              reads=[thr, C32], writes=[dthr])
                        sc.op("pe", lambda e: e.matmul(PS[6].t[:, 0:128], lhsT=ONESF, rhs=dthr.t[:, :], start=True, stop=True),
                              reads=[dthr, C32], writes=[PS[6]])
                        sc.op("dve", lambda e: e.tensor_copy(out=thrbc.t[:, :], in_=PS[6].t[:, 0:128]), reads=[PS[6]], writes=[thrbc])
                        for c in range(nch):
                            k0 = 4 + 4 * c
                            nt_ = min(4, Tq + 1 - k0)
                            ps = PS[c % 2]
                            for t_ in range(nt_):
                                sc.op("pe", lambda e: e.transpose(out=ps.t[:, t_ * 128:(t_ + 1) * 128],
                                                                  in_=SCR.t[:, (k0 - 4 + t_) * 128:(k0 - 3 + t_) * 128], identity=IDF),
                                      reads=[SCR, C32], writes=[ps])
                            sc.op("dve", lambda e: e.tensor_tensor(out=NS.t[:, k0:k0 + nt_, :],
                                                                   in0=ps.t[:, :nt_ * 128].rearrange("p (t q) -> p t q", t=nt_),
                                                                   in1=thrbc.t[:, :].unsqueeze(1).to_broadcast([128, nt_, 128]), op=ALU.is_lt),
                                  reads=[ps, thrbc], awrites=[NS])

                    def stage3(v):
                        Tq, nkt, N, nch, iq_, q_, g_, o_, tcol, si, ql = (v[k] for k in ("Tq", "nkt", "N", "nch", "iq_", "q_", "g_", "o_", "tcol", "si", "ql"))
                        units = [(kt, kv) for kt in range(4, Tq + 1) for kv in range(2)]
                        vcur = {}

                        def qk(u, pidx):
                            kt, kv = u
                            ps = PS[pidx % 4]
                            dd = Tq - kt
                            sc.op("pe", lambda e: e.matmul(ps.t[:, :].rearrange("p (h q) -> p h q", h=4), lhsT=kb.t[:64, kv, kt * 128:(kt + 1) * 128],
                                                           rhs=q_.t[:, 4 * kv:4 * kv + 4, :], start=True, stop=False), reads=[kb, q_], writes=[ps])
                            sc.op("pe", lambda e: e.matmul(ps.t[:, :].rearrange("p (h q) -> p h q", h=4), lhsT=INEG,
                                                           rhs=NS.t[:, kt, :].unsqueeze(1).to_broadcast([128, 4, 128]), start=False, stop=(dd >= 5)),
                                  reads=[CBF, NS], writes=[ps])
                            if dd < 5:
                                sc.op("pe", lambda e: e.matmul(ps.t[:, :].rearrange("p (h q) -> p h q", h=4), lhsT=I8,
                                                               rhs=BMB.t[:, dd, 4 * kv:4 * kv + 4, :], start=False, stop=True),
                                      reads=[CBF, BMB], writes=[ps])
                        def load_chunk(k0):
                            vb_ = vbc[vci_box[0] % 4]; vci_box[0] += 1
                            ntl = min(4, Tq + 1 - k0)
                            sc.dma("sp", vb_.t[:, :ntl, :], VV[k0 * 128:(k0 + ntl) * 128, 128:384].rearrange("(t p) x -> p t x", p=128), vb_,
                                   reads=[dVV], writes=[vb_])
                            return vb_
                        qk(units[0], 0)
                        for n_, (kt, kv) in enumerate(units):
                            if n_ + 1 < len(units):
                                qk(units[n_ + 1], n_ + 1)
                            if (kt - 4) % 4 == 0 and kv == 0:
                                if kt == 4:
                                    vcur[4] = load_chunk(4)
                                if kt + 4 <= Tq:
                                    vcur[kt + 4] = load_chunk(kt + 4)
                                vcur["b"] = vcur[kt]; vcur["k0"] = kt
                            vb_ = vcur["b"]
                            ps = PS[n_ % 4]; p_ = pt[n_ % 4]
                            acc = PS[4 + kv]
                            sc.op("act", lambda e: e.activation(out=p_.t[:, :], in_=ps.t[:, :], func=AF.Exp, scale=0.125,
                                                                bias=KBIAS[:, kt:kt + 1]), reads=[ps, C32], writes=[p_])
                            sc.op("pe", lambda e: e.matmul(acc.t[:, :], lhsT=vb_.t[:, kt - vcur["k0"], kv * 128:(kv + 1) * 128], rhs=p_.t[:, :],
                                                           start=(kt == 4), stop=(kt == Tq)), reads=[vb_, p_], writes=[acc])
                        for kv in range(2):
                            attn_epilogue(PS[4 + kv], T1, PS[6], tmps, g_.t[:, 4 * kv:4 * kv + 4, :], g_, o_.t[:, 4 * kv:4 * kv + 4, :], o_)
                        sc.dma("pool", BR[512:1024, tcol].rearrange("(h d) t -> d h t", d=64), o_.t[:, :, :], o_, reads=[o_], awrites=[dBR])
                    QL = [qvars(si, g, ql, i4 * 4 + ql) for i4, (si, g) in enumerate(enumerate(own)) for ql in range(4)]
                    stage1(QL[0])
                    for n_q, v in enumerate(QL):
                        stage2(v)
                        if n_q + 1 < len(QL):
                            stage1(QL[n_q + 1])
                        stage3(v)
                    sc.barrier()

                with ExitStack() as st:
                    wb = sb(st, "wb", [128, 12, D], BF16)
                    wo = sb(st, "wo", [128, 8, D], BF16)
                    wst = [sb(st, "wst3_%d" % i, [128, 4, 512]) for i in range(2)]
                    br = [sb(st, "br%d" % i, [128, 12, 512], BF16) for i in range(2)]
                    mg = [sb(st, "mg%d" % i, [128, 3, 512], BF16) for i in range(2)]
                    tm = [sb(st, "tm%d" % i, [128, 512]) for i in range(3)]
                    mgd = [sb(st, "mgd%d" % i, [128, 8, 512], BF16) for i in range(2)]
                    xt = [sb(st, "x3_%d" % i, [128, D]) for i in range(2)]
                    xo = [sb(st, "xo%d" % i, [128, D]) for i in range(2)]
                    ci = 0
                    wbv = w_br[l].rearrange("n (c p) d -> p (n c) d", p=128)
                    wov = w_out[l].rearrange("(c p) d -> p c d", p=128)
                    for (srcv, dstb, nchunk) in [(wbv, wb, 12), (wov, wo, 8)]:
                        for c4 in range(0, nchunk, 4):
                            for d0 in (0, 512):
                                s_ = wst[ci % 2]
                                sc.dma("sp", s_.t[:, :, :], srcv[:, c4:c4 + 4, d0:d0 + 512], s_, writes=[s_])
                                en = ["dve", "pool"][ci % 2]; ci += 1
                                sc.op(en, lambda e: e.tensor_copy(out=dstb.t[:, c4:c4 + 4, d0:d0 + 512], in_=s_.t[:, :, :]), reads=[s_], awrites=[dstb])
                    for si, g in enumerate(own):
                        tq = slice(si * 512, (si + 1) * 512)
                        b_, md = br[si % 2], mgd[si % 2]
                        sc.dma("sp", b_.t[:, :, :], BR[:, tq].rearrange("(c p) t -> p c t", p=128), b_, reads=[dBR], writes=[b_])
                        for dc in range(8):
                            m_ = mg[dc % 2]
                            sc.dma("sp", m_.t[:, :, :], MT[:, tq].rearrange("(n c p) t -> p n c t", p=128, n=3)[:, :, dc, :], m_, reads=[dMT], writes=[m_])
                            for n in range(3):
                                ps = PS[n]
                                for c in range(4):
                                    sc.op("pe", lambda e: e.matmul(ps.t[:, :], lhsT=wb.t[:, 4 * n + c, dc * 128:(dc + 1) * 128], rhs=b_.t[:, 4 * n + c, :],
                                                                   start=(c == 0), stop=(c == 3)), reads=[wb, b_], writes=[ps])
                                sc.op("dve", lambda e: e.tensor_tensor(out=tm[n].t[:, :], in0=ps.t[:, :], in1=m_.t[:, n, :], op=ALU.mult),
                                      reads=[ps, m_], writes=[tm[n]])
                            sc.op("pool", lambda e: e.tensor_tensor(out=tm[0].t[:, :], in0=tm[0].t[:, :], in1=tm[1].t[:, :], op=ALU.add),
                                  reads=[tm[0], tm[1]], writes=[tm[0]])
                            sc.op("pool", lambda e: e.tensor_tensor(out=md.t[:, dc, :], in0=tm[0].t[:, :], in1=tm[2].t[:, :], op=ALU.add),
                                  reads=[tm[0], tm[2]], awrites=[md])
                        for tt in range(4):
                            tile = 4 * g + tt
                            x_, xo_ = xt[tt % 2], xo[tt % 2]
                            sc.dma("sp", x_.t[:, :], xsrc[tile * 128:(tile + 1) * 128, :], x_, reads=([dXS] if dXS else []), writes=[x_])
                            for e2 in range(2):
                                ps = PS[4 + e2]
                                for dc in range(8):
                                    sc.op("pe", lambda e: e.matmul(ps.t[:, :], lhsT=md.t[:, dc, tt * 128:(tt + 1) * 128], rhs=wo.t[:, dc, e2 * 512:(e2 + 1) * 512],
                                                                   start=(dc == 0), stop=(dc == 7)), reads=[md, wo], writes=[ps])
                                sc.op("dve", lambda e: e.tensor_tensor(out=xo_.t[:, e2 * 512:(e2 + 1) * 512], in0=ps.t[:, :], in1=x_.t[:, e2 * 512:(e2 + 1) * 512],
                                                                       op=ALU.add), reads=[ps, x_], awrites=[xo_])
                            if l < depth - 1:
                                sc.dma("pool", X1[tile * 128:(tile + 1) * 128, :], xo_.t[:, :], xo_, reads=[xo_], awrites=[dX1])
                            else:
                                sc.dma("pool", y[(si * 4 + tt) * 128:(si * 4 + tt + 1) * 128, :], xo_.t[:, :], xo_, reads=[xo_], awrites=[dY])
                    sc.barrier()
    return nc, dict(NG=NG, SP=SP, NT=NT, NOUT=NOUT, NC32=NC32, NCBF=NCBF, ninst=sc.ninst, nsem=sc.nsem)


def _t5_bucket_np(delta):
    n = np.maximum(delta, 0)
    nf = np.maximum(n, 1).astype(np.float32)
    large = 16 + (np.log(nf / np.float32(16)) / np.float32(math.log(512 / 16)) * np.float32(16)).astype(np.int32)
    large = np.minimum(large, 31)
    return np.where(n < 16, n, large)


def _host_consts(S, j):
    NGP = S // 512 + 1
    NT = NGP * 4
    pad = 1024 - 512 * j
    k = np.arange(128)[:, None]
    q = np.arange(128)[None, :]
    idf = np.eye(128, dtype=np.float32)
    ones = np.ones((128, 128), np.float32)
    bd = np.zeros((128, 128), np.float32); bd[:64, :64] = 1 / 64; bd[64:, 64:] = 1 / 64
    e0 = np.zeros((128, 128), np.float32); e0[0, :] = 1
    sh = np.zeros((128, 64), np.float32); sh[64 + np.arange(64), np.arange(64)] = 1
    caus = np.where(q > k, np.float32(-1e30), np.float32(0)).astype(np.float32)
    kbias = np.zeros((128, NT), np.float32); kbias[:, :pad // 128] = -30000.0
    kpos = 512 + np.arange(512)
    kpad = np.tile(np.where(kpos < pad, np.float32(-1e30), np.float32(0))[None, :], (128, 1)).astype(np.float32)
    c32 = np.concatenate([idf, ones, bd, e0, sh, caus, kbias, kpad], axis=1).astype(np.float32)
    allpos = np.arange(NGP * 512)
    kbrow = np.where(allpos < pad, np.float32(-1e30), (-(allpos.astype(np.float64)) * 1e-30).astype(np.float32))[None, :].astype(np.float32)
    i8 = 8.0 * idf
    ineg = -30000.0 * idf
    ma0 = np.where(k <= q, 0.0, NEGM)
    ma1 = np.where(k > q, 0.0, NEGM)
    cms = []
    qq = np.arange(512)[None, :]
    for i in range(4):
        cms.append(np.where(128 * i + k <= qq, 0.0, NEGM))
    cbf = np.concatenate([i8, ineg, ma0, ma1] + cms + [idf], axis=1).astype(ml_dtypes.bfloat16)
    return c32, cbf, kbrow


def _t5_tiles(rel_bias_cols, dds):
    k = np.arange(128)[:, None]
    q = np.arange(128)[None, :]
    out = []
    for dd in dds:
        if dd is None:
            b = np.full((128, 128), 31, np.int64)
        else:
            b = _t5_bucket_np(dd * 128 + q - k)
        t = rel_bias_cols[b]
        out.append(np.ascontiguousarray(t.transpose(0, 2, 1)).reshape(128, 1024))
    return np.stack(out).astype(np.float32)


def make_in_maps(S, x, norm_gain, w_in, b_forget, qk_gain, sinks, w_branch, w_out, rel_bias):
    B = x.shape[0]
    depth = w_in.shape[0]
    SP = S + 512
    f = lambda a: np.ascontiguousarray(a, dtype=np.float32)
    gainbc = f(np.broadcast_to(norm_gain[:, None, :], (depth, 128, D)))
    qkg = np.zeros((depth, 128, 8), np.float32)
    for n in range(3):
        for r in range(2):
            qkg[:, :, 2 * n + r] = np.tile(qk_gain[:, n, r, :], (1, 2))
    bfc = np.zeros((depth, 128, 1), np.float32); bfc[:, :8, 0] = b_forget
    sinkrep = f(np.broadcast_to(np.repeat(sinks, 128, axis=1)[:, None, :], (depth, 128, 1024)))
    t5a = _t5_tiles(np.asarray(rel_bias)[:, :8], [0, 1])
    t5b = _t5_tiles(np.asarray(rel_bias)[:, 8:], [0, 1, 2, 3, 4, None])
    consts = [_host_consts(S, j) for j in range(2)]
    maps = []
    for b in range(B):
        for j in range(2):
            pad = 1024 - 512 * j
            ntok = SP - pad
            xs_ = np.zeros((SP, D), np.float32)
            xs_[pad:pad + ntok] = x[b, :ntok]
            maps.append({"xs": xs_, "w_in": f(w_in), "w_br": f(w_branch), "w_out": f(w_out), "gainbc": gainbc, "qkg": qkg,
                         "bfc": bfc, "sinkrep": sinkrep, "t5a": t5a, "t5b": t5b, "c32": consts[j][0], "cbf": consts[j][1], "kbrow": consts[j][2]})
    return maps


def assemble(S, B, results):
    NG = S // 512
    out = np.zeros((B, S, D), np.float32)
    for b in range(B):
        for j in range(2):
            yv = np.asarray(results[2 * b + j]["y"])
            for si in range(NG // 2):
                gg = 2 * si + j
                out[b, gg * 512:(gg + 1) * 512] = yv[si * 512:(si + 1) * 512]
    return out


_CACHE = {}


def kernel(x, norm_gain, w_in, b_forget, qk_gain, sinks, w_branch, w_out, rel_bias):
    x = np.asarray(x, np.float32)
    B, S, _ = x.shape
    args = [np.asarray(a, np.float32) for a in (norm_gain, w_in, b_forget, qk_gain, sinks, w_branch, w_out, rel_bias)]
    if S not in _CACHE:
        _CACHE[S] = build_program(S, depth=args[1].shape[0])
    nc, info = _CACHE[S]
    maps = make_in_maps(S, x, *args)
    res = run_bass_kernel_spmd(nc, maps, core_ids=list(range(len(maps))))
    return assemble(S, B, res.results)
```
